# Optimizing a Trainium2 kernel written in Bass

```python
import math
import jax, jax.numpy as jnp
from jax import lax
import numpy as np

D_MODEL = 1024
BATCH = 8
SEQ = 2048
DEPTH = 4

N_HEADS = 16
HEAD_DIM = D_MODEL // N_HEADS
D_FF = 4 * D_MODEL
N_A_LAYERS = DEPTH // 2
N_B_LAYERS = DEPTH - N_A_LAYERS
MOBA_BLOCK = 256
MOBA_TOPK = 3
MOBA_Q_CHUNK = 16
FOX_Q_BLOCK = 128
RMS_EPS = 1e-6
NEG_INF = -1e30

kernel_name = "yoco_moba_fox_hybrid"


def rms_norm(x, g):
    xf = x.astype(jnp.float32)
    y = xf * lax.rsqrt(jnp.mean(xf * xf, axis=-1, keepdims=True) + RMS_EPS)
    return (y * g.astype(jnp.float32)).astype(x.dtype)


def alibi_slopes(n):
    return jnp.asarray(2.0 ** (-8.0 * np.arange(1, n + 1) / n), dtype=jnp.float32)


def split_heads(t):
    b, s, _ = t.shape
    return t.reshape(b, s, N_HEADS, HEAD_DIM).transpose(0, 2, 1, 3)


def merge_heads(t):
    b, h, s, d = t.shape
    return t.transpose(0, 2, 1, 3).reshape(b, s, h * d)


def squared_relu_mlp(x, w_up, w_down):
    return jnp.square(jax.nn.relu(x @ w_up)) @ w_down


def moba_attention(q, k, v):
    b, h, s, d = q.shape
    nb = -(-s // MOBA_BLOCK)
    pad = ((0, 0), (0, 0), (0, nb * MOBA_BLOCK - s), (0, 0))
    kb = jnp.pad(k, pad).reshape(b, h, nb, MOBA_BLOCK, d)
    vb = jnp.pad(v, pad).reshape(b, h, nb, MOBA_BLOCK, d)
    scale = 1.0 / math.sqrt(d)
    slopes = alibi_slopes(h)[None, :, None, None]
    n_sel = min(MOBA_TOPK, nb - 1)
    own_blk = jnp.arange(s) // MOBA_BLOCK
    bi = jnp.arange(b)[:, None, None, None]
    hi = jnp.arange(h)[None, :, None, None]

    if n_sel > 0:
        k_mean = jnp.mean(kb.astype(jnp.float32), axis=3)
        gate = jnp.einsum('bhsd,bhnd->bhsn', q.astype(jnp.float32), k_mean)
        past = jnp.arange(nb)[None, :] < own_blk[:, None]
        gate = jnp.where(past, gate, NEG_INF)
        _, sel = lax.top_k(gate, n_sel)
        sel_valid = jnp.arange(n_sel)[None, :] < own_blk[:, None]

    n_chunks = s // MOBA_Q_CHUNK
    blk_offs = jnp.arange(MOBA_BLOCK)

    def chunk(ci):
        t0 = ci * MOBA_Q_CHUNK
        qc = lax.dynamic_slice_in_dim(q, t0, MOBA_Q_CHUNK, axis=2)
        tq = t0 + jnp.arange(MOBA_Q_CHUNK)
        blk = t0 // MOBA_BLOCK
        k_own = lax.dynamic_index_in_dim(kb, blk, axis=2, keepdims=False)
        v_own = lax.dynamic_index_in_dim(vb, blk, axis=2, keepdims=False)
        ks_own = blk * MOBA_BLOCK + blk_offs
        dist_own = (tq[:, None] - ks_own[None, :]).astype(jnp.float32)
        s_own = (jnp.einsum('bhcd,bhkd->bhck', qc, k_own).astype(jnp.float32) * scale
                 - slopes * dist_own)
        s_own = jnp.where(dist_own >= 0, s_own, NEG_INF)
        if n_sel > 0:
            sel_c = lax.dynamic_slice_in_dim(sel, t0, MOBA_Q_CHUNK, axis=2)
            valid_c = lax.dynamic_slice_in_dim(sel_valid, t0, MOBA_Q_CHUNK, axis=0)
            k_sel = kb[bi, hi, sel_c]
            v_sel = vb[bi, hi, sel_c]
            ks_sel = sel_c[..., None] * MOBA_BLOCK + blk_offs
            dist_sel = (tq[:, None, None] - ks_sel).astype(jnp.float32)
            s_sel = (jnp.einsum('bhcd,bhcnkd->bhcnk', qc, k_sel).astype(jnp.float32) * scale
                     - slopes[..., None] * dist_sel)
            s_sel = jnp.where(valid_c[:, :, None], s_sel, NEG_INF)
            scores = jnp.concatenate(
                [s_sel.reshape(b, h, MOBA_Q_CHUNK, n_sel * MOBA_BLOCK), s_own], axis=-1)
            p = jax.nn.softmax(scores, axis=-1)
            p_sel = p[..., :n_sel * MOBA_BLOCK].reshape(b, h, MOBA_Q_CHUNK, n_sel, MOBA_BLOCK).astype(v.dtype)
            p_own = p[..., n_sel * MOBA_BLOCK:].astype(v.dtype)
            return (jnp.einsum('bhcnk,bhcnkd->bhcd', p_sel, v_sel)
                    + jnp.einsum('bhck,bhkd->bhcd', p_own, v_own))
        p_own = jax.nn.softmax(s_own, axis=-1).astype(v.dtype)
        return jnp.einsum('bhck,bhkd->bhcd', p_own, v_own)

    outs = lax.map(chunk, jnp.arange(n_chunks))
    return outs.transpose(1, 2, 0, 3, 4).reshape(b, h, s, d)


def shared_kv(h, kv_norm, w_kv, w_f, b_f):
    hn = rms_norm(h, kv_norm)
    k, v = jnp.split(hn @ w_kv, 2, axis=-1)
    log_f = jax.nn.log_sigmoid((hn @ w_f + b_f).astype(jnp.float32))
    c = jnp.cumsum(log_f, axis=1).transpose(0, 2, 1)
    return split_heads(k), split_heads(v), c


def forgetting_attention(q, k, v, c):
    b, h, s, d = q.shape
    scale = 1.0 / math.sqrt(d)
    kpos = jnp.arange(s)

    def block(qi):
        t0 = qi * FOX_Q_BLOCK
        qb = lax.dynamic_slice_in_dim(q, t0, FOX_Q_BLOCK, axis=2)
        cq = lax.dynamic_slice_in_dim(c, t0, FOX_Q_BLOCK, axis=2)
        tq = t0 + jnp.arange(FOX_Q_BLOCK)
        sc = (jnp.einsum('bhqd,bhkd->bhqk', qb, k).astype(jnp.float32) * scale
              + cq[..., None] - c[:, :, None, :])
        sc = jnp.where(kpos[None, :] <= tq[:, None], sc, NEG_INF)
        p = jax.nn.softmax(sc, axis=-1).astype(v.dtype)
        return jnp.einsum('bhqk,bhkd->bhqd', p, v)

    outs = lax.map(block, jnp.arange(s // FOX_Q_BLOCK))
    return outs.transpose(1, 2, 0, 3, 4).reshape(b, h, s, d)


def setup_inputs(seed: int = 0) -> dict:
    key = jax.random.key(seed)
    ks = jax.random.split(key, 14)
    D, F, H = D_MODEL, D_FF, N_HEADS
    nrm = lambda k, shape, fan_in, gain=1.0: (gain * fan_in ** -0.5) * jax.random.normal(k, shape, jnp.float32)
    return {
        "x": jax.random.normal(ks[0], (BATCH, SEQ, D), jnp.float32),
        "attn_norm": 1.0 + 0.02 * jax.random.normal(ks[1], (DEPTH, D), jnp.float32),
        "moba_w_qkv": nrm(ks[2], (N_A_LAYERS, D, 3 * D), D),
        "fox_w_q": nrm(ks[3], (N_B_LAYERS, D, D), D),
        "w_o": nrm(ks[4], (DEPTH, D, D), D, 0.5),
        "kv_norm": 1.0 + 0.02 * jax.random.normal(ks[5], (D,), jnp.float32),
        "w_kv": nrm(ks[6], (D, 2 * D), D),
        "w_f": nrm(ks[7], (D, H), D),
        "b_f": 3.0 + 0.5 * jax.random.normal(ks[8], (H,), jnp.float32),
        "mlp_norm": 1.0 + 0.02 * jax.random.normal(ks[9], (DEPTH, D), jnp.float32),
        "w_up": nrm(ks[10], (DEPTH, D, F), D),
        "w_down": nrm(ks[11], (DEPTH, F, D), F, 0.5),
        "final_norm": 1.0 + 0.02 * jax.random.normal(ks[12], (D,), jnp.float32),
    }


def reference(x, attn_norm, moba_w_qkv, fox_w_q, w_o, kv_norm, w_kv, w_f, b_f,
              mlp_norm, w_up, w_down, final_norm):
    h = x
    k_sh = v_sh = c_sh = None
    for layer in range(DEPTH):
        if layer == N_A_LAYERS:
            k_sh, v_sh, c_sh = shared_kv(h, kv_norm, w_kv, w_f, b_f)
        hn = rms_norm(h, attn_norm[layer])
        if layer < N_A_LAYERS:
            q, k, v = jnp.split(hn @ moba_w_qkv[layer], 3, axis=-1)
            mix = moba_attention(split_heads(q), split_heads(k), split_heads(v))
        else:
            q = split_heads(hn @ fox_w_q[layer - N_A_LAYERS])
            mix = forgetting_attention(q, k_sh, v_sh, c_sh)
        h = h + merge_heads(mix) @ w_o[layer]
        h = h + squared_relu_mlp(rms_norm(h, mlp_norm[layer]), w_up[layer], w_down[layer])
    return rms_norm(h, final_norm)
```

```python
import contextlib
import numpy as np
import ml_dtypes
import concourse.bass as bass
import concourse.mybir as mybir
from concourse.bass_utils import run_bass_kernel_spmd

F32 = mybir.dt.float32
BF16 = mybir.dt.bfloat16
AF = mybir.ActivationFunctionType
ALU = mybir.AluOpType
AX = mybir.AxisListType

S = 2048
D = 1024
H = 16
DH = 64
FF = 4096
NT = 16
NB = 4
KC = 8
NSLOT = 4
SLOT = 4096
NEG = -30000.0
EPS = 1e-6
SCALE = 0.125
N_A = 2
DEPTH = 4


class Eng:
    def __init__(self, name, h):
        self.name = name
        self.h = h
        self.nsig = 0
        self.seen = {}


class B:
    def __init__(self, stop=None):
        self.stop = stop
        import os as _os
        self.pair_order = list(range(8))[::-1] if _os.environ.get("KREV") else list(range(8))
        self.nc = bass.Bass("TRN2", target_bir_lowering=False)
        self.es = contextlib.ExitStack()
        nc = self.nc
        self.engs = {"pe": Eng("pe", nc.tensor), "act": Eng("act", nc.scalar), "dve": Eng("dve", nc.vector),
                     "pool": Eng("pool", nc.gpsimd), "sp": Eng("sp", nc.sync)}
        self.sems = {}
        self.dcount = {}
        for n in self.engs:
            self.sems[n] = self.es.enter_context(nc.semaphore("s_" + n))
        self.res = {}
        self.const_keys = set()
        self.gbank = 0

    def dsem(self, name):
        if name not in self.sems:
            self.sems[name] = self.es.enter_context(self.nc.semaphore("d_" + name))
            self.dcount[name] = 0
        return name

    def sb(self, name, shape, dt):
        return self.es.enter_context(self.nc.sbuf_tensor(name, shape, dt))

    def _wait(self, eng, ev):
        key, val = ev
        if eng.seen.get(key, 0) >= val:
            return
        eng.h.wait_ge(self.sems[key], val)
        eng.seen[key] = val

    def op(self, en, fn, reads=(), writes=(), signal=True, dsem=None):
        eng = self.engs[en]
        evs = []
        for r in reads:
            st = self.res.get(r)
            if st is not None and st[0] is not None:
                evs.append(st[0])
        for w in writes:
            st = self.res.get(w)
            if st is not None:
                if st[0] is not None:
                    evs.append(st[0])
                evs.extend(st[1].items())
        for ev in evs:
            if ev[0] == "pe" and en == "pe":
                continue
            if ev[0] in self.engs:
                assert self.engs[ev[0]].nsig >= ev[1], ("unsignaled dependency", en, ev, reads, writes)
            self._wait(eng, ev)
        ins = fn(eng.h)
        if dsem is not None:
            self.dsem(dsem)
            ins.then_inc(self.sems[dsem], 16)
            self.dcount[dsem] += 16
            ev = (dsem, self.dcount[dsem])
        elif signal:
            ins.then_inc(self.sems[en], 1)
            eng.nsig += 1
            ev = (en, eng.nsig)
        else:
            ev = (en, eng.nsig + 1)
        for w in writes:
            self.res[w] = [ev, {}]
        for r in reads:
            if r in self.const_keys:
                continue
            st = self.res.setdefault(r, [None, {}])
            if st[1].get(ev[0], 0) < ev[1]:
                st[1][ev[0]] = ev[1]
        return ev

    def galloc(self):
        b = 4 + self.gbank
        self.gbank = (self.gbank + 1) % 4
        return b

    def declare(self):
        nc = self.nc
        di = lambda n, s, d=F32: nc.dram_tensor(n, s, d, kind="ExternalInput").ap()
        self.x = di("x", [S, D])
        self.w_qkv = di("wqkv_r", [N_A, 8, 128, 3 * KC * 128])
        self.w_q = di("wq_r", [DEPTH - N_A, 8, 128, KC * 128])
        self.w_o = di("w_o", [DEPTH, D, D])
        self.w_kv = di("wkv_r", [8, 128, 2 * KC * 128])
        self.w_f = di("wf_r", [128, KC * H])
        self.b_f = di("b_f", [H, 1])
        self.w_up = di("w_up", [DEPTH, D, FF])
        self.w_down = di("w_down", [DEPTH, FF, D])
        self.final_norm = di("final_norm", [D])
        self.gtab_d = di("gtab", [128, 9, KC])
        self.identf_d = di("identf", [128, 128])
        self.identb_d = di("identb", [128, 128], BF16)
        self.trimask_d = di("trimask", [128, 128], BF16)
        self.kblock_d = di("kblock", [8, S], BF16)
        self.abias_d = di("abias", [128, NT, H])
        self.augq_d = di("augq", [H, 8, S], BF16)
        self.gmask_d = di("gmask", [128, 128])
        self.gvalid_d = di("gvalid", [128, 128])
        self.Ks = nc.dram_tensor("Ks", [H, 65, S], BF16, kind="Internal").ap()
        self.Vs = nc.dram_tensor("Vs", [H // 2, 128, NT * 192], BF16, kind="Internal").ap()
        self.Cs = nc.dram_tensor("Cs", [H, S], BF16, kind="Internal").ap()
        self.out = nc.dram_tensor("out", [S, D], F32, kind="ExternalOutput").ap()

        sb = self.sb
        self.h = sb("h", [128, NT, D], F32)
        self.hnT = sb("hnT", [128, KC, S], BF16)
        self.big = sb("big", [128, 16384], BF16)
        self.ring = sb("ring", [128, NSLOT * SLOT], BF16)
        self.QTall = sb("QTall", [128, 2 * S], BF16)
        self.KTall = sb("KTall", [128, 2 * S], BF16)
        self.Vp = sb("Vp", [128, NT, 192], BF16)
        self.PT = sb("PT", [128, 4, 512], BF16)
        self.rc = sb("rc", [128, 2, 512], F32)
        self.gfin = sb("gfin", [128, D], F32)
        self.gT = sb("gT", [128, 9, KC], F32)
        self.identf = sb("identf_s", [128, 128], F32)
        self.identb = sb("identb_s", [128, 128], BF16)
        self.trimask = sb("trimask_s", [128, 128], BF16)
        self.abias = sb("abias_s", [128, NT, H], F32)
        self.cbias = sb("cbias_s", [128, NT, H], F32)
        self.gmask = sb("gmask_s", [128, 128], F32)
        self.gvalid = sb("gvalid_s", [128, 128], F32)
        self.ss = sb("ss", [128, NT], F32)
        self.rt = sb("rt", [128, NT], F32)
        self.rstd = sb("rstd", [128, NT], F32)
        self.bft = sb("bft", [H, 1], F32)
        self.nbf = sb("nbf", [H, 1], F32)
        self.kmf = sb("kmf", [64, 2, 8], F32)
        self.kmT = sb("kmT", [64, 2, 8], BF16)
        self.g2 = sb("g2", [128, 128], F32)
        self.top8 = sb("top8", [128, 128], F32)
        self.selt = sb("selt", [128, 128], F32)
        self.augtok = sb("augtok", [128, 64 + 128], F32)
        self.onesr = sb("onesr", [1, S], BF16)
        self.ps = [self.es.enter_context(nc.psum_tensor(f"ps{b}", [128, 512], F32)) for b in range(8)]

        self.QT = [self.QTall[:, i * S:(i + 1) * S] for i in range(2)]
        self.KT = [self.KTall[:, i * S:(i + 1) * S] for i in range(2)]
        qf = self.QTall[:, :].bitcast(F32)
        self.xs = [qf[:, k * 1024:(k + 1) * 1024] for k in range(2)]
        self.mixT = self.big[:, :].rearrange("p (c t) -> p c t", t=S)
        self.uT = [self.big[:, b * 8192:(b + 1) * 8192].rearrange("p (f t) -> p f t", t=S) for b in range(2)]
        self.bigf = self.big[:, :].bitcast(F32)
        self.XS_KEYS = [[("QT", k, tb) for tb in range(NB)] + [("QTaug", k)] for k in range(2)]
        self.BIG_KEYS = ([("uT", b, fs, tb) for b in range(2) for fs in range(4) for tb in range(NB)]
                         + [("mixT", p, i, j) for p in range(8) for i in range(2) for j in range(NB)])

    def setup(self):
        c = self.dsem("c")
        consts = [
            (self.identf[:], self.identf_d, "identf"), (self.identb[:], self.identb_d, "identb"),
            (self.trimask[:], self.trimask_d, "trimask"), (self.abias[:], self.abias_d, "abias"),
            (self.gmask[:], self.gmask_d, "gmask"), (self.gvalid[:], self.gvalid_d, "gvalid"),
            (self.gT[:], self.gtab_d, "gT"), (self.gfin[:], self.final_norm.partition_broadcast(128), "gfin"),
            (self.bft[:], self.b_f, "bft"),
            (self.KT[0][64:72, :], self.kblock_d, "KTaug0"), (self.KT[1][64:72, :], self.kblock_d, "KTaug1"),
        ]
        for (o, i, k) in consts:
            self.op("sp", lambda e, o=o, i=i: e.dma_start(out=o, in_=i), writes=[k], dsem=c)
        tot = self.dcount[c]
        for (_, _, k) in consts:
            self.res[k] = [(c, tot), {}]
            if k != "bft":
                self.const_keys.add(k)
        xv = self.x.rearrange("(t p) f -> p t f", p=128)
        for g in range(4):
            self.op("sp", lambda e, g=g: e.dma_start(out=self.h[:, 4 * g:4 * g + 4, :], in_=xv[:, 4 * g:4 * g + 4, :]),
                    writes=[("h", tt) for tt in range(4 * g, 4 * g + 4)], dsem=f"x{g}")
        self.op("dve", lambda e: e.memset(self.Vp[:, :, 64:128], 1.0), writes=["Vones"])
        self.op("dve", lambda e: e.memset(self.augtok[:, 0:64], 0.0), writes=["augpad"])
        self.op("dve", lambda e: e.memset(self.onesr[:], 1.0), writes=["onesr"])
        self.op("dve", lambda e: e.tensor_scalar(out=self.nbf[:], in0=self.bft[:], scalar1=-1.0, scalar2=None,
                                                 op0=ALU.mult), reads=["bft"], writes=["nbf"])
        for k in ("Vones", "augpad", "onesr", "nbf"):
            self.const_keys.add(k)

    def ring_view(self, s, kc, f):
        return self.ring[:, s * SLOT:s * SLOT + kc * f].rearrange("p (k f) -> p k f", f=f)

    def build_wlist(self):
        wl = []
        rr = lambda ap: ap.rearrange("(k p) f -> p k f", p=128)

        def mlp(L):
            ups = [("up", [(0, (KC, 512), None, rr(self.w_up[L])[:, :, c * 512:(c + 1) * 512])]) for c in range(8)]
            dns = [("down", [(0, (4, D), None, rr(self.w_down[L][c * 512:(c + 1) * 512, :]))]) for c in range(8)]
            order = [ups[0]]
            for c in range(8):
                if c + 1 < 8:
                    order.append(ups[c + 1])
                order.append(dns[c])
            return order

        def wo(L):
            return [("wo", [(0, (KC, 512), None, rr(self.w_o[L])[:, :, hf * 512:(hf + 1) * 512])]) for hf in range(2)]

        for L in range(N_A):
            for p in self.pair_order:
                wl.append(("qkv", [((0, 1, 2), 3 * KC * 128, 3, self.w_qkv[L, p])]))
            wl += wo(L) + mlp(L)
        for p in range(8):
            wl.append(("kv", [((0, 1), 2 * KC * 128, 2, self.w_kv[p])]))
        wl.append(("wf", [((0,), KC * H, 0, self.w_f)]))
        for L in range(N_A, DEPTH):
            for p in range(8):
                wl.append(("q", [((0,), KC * 128, 1, self.w_q[L - N_A, p])]))
            wl += wo(L) + mlp(L)
        self.wl = wl
        self.wpos = 0
        self.wfill = 0
        for _ in range(NSLOT):
            self.fill_next()

    def fill_next(self):
        if self.wfill >= len(self.wl):
            return
        i = self.wfill
        self.wfill += 1
        s = i % NSLOT
        name, parts = self.wl[i]
        for (part, sz, nt, src) in parts:
            if nt is None:
                kc, f = sz
                dst = self.ring_view(s, kc, f)
            else:
                dst = self.ring[:, s * SLOT:s * SLOT + sz]
            keys = [("ring", s, t) for t in range(3)]
            self.op("pool", lambda e, dst=dst, src=src: e.dma_start(out=dst, in_=src), writes=keys, dsem=f"r{s}")

    def part_view(self, s, t):
        return self.ring[:, s * SLOT + t * 1024:s * SLOT + (t + 1) * 1024].rearrange("p (k c) -> p k c", c=128)

    def wget(self, name):
        i = self.wpos
        self.wpos += 1
        assert self.wl[i][0] == name, (self.wl[i][0], name)
        s = i % NSLOT
        parts = self.wl[i][1]
        nt = parts[0][2]
        if nt is not None:
            if nt == 0:
                return s, self.ring[:, s * SLOT:s * SLOT + KC * H].rearrange("p (k h) -> p k h", h=H)
            return s, [self.part_view(s, t) for t in range(nt)]
        kc, f = parts[0][1]
        return s, self.ring_view(s, kc, f)

    def wdone(self):
        self.fill_next()

    def stats(self):
        self.op("dve", lambda e: e.memset(self.ss[:], 0.0), writes=["ss"])
        for tt in range(NT):
            k = tt % 2
            self.op("act", lambda e, tt=tt, k=k: e.activation(out=self.xs[k], in_=self.h[:, tt, :], func=AF.Square,
                                                              accum_out=self.ss[:, tt:tt + 1]),
                    reads=[("h", tt), "ss"], writes=self.XS_KEYS[k] + [("ssc", tt)])
        self.op("act", lambda e: e.activation(out=self.rt[:], in_=self.ss[:], func=AF.Sqrt, bias=EPS, scale=1.0 / D),
                reads=[("ssc", tt) for tt in range(NT)] + ["ss"], writes=["rt"])
        self.op("dve", lambda e: e.reciprocal(out=self.rstd[:], in_=self.rt[:]), reads=["rt"], writes=["rstd"])

    def norm(self, gi):
        self.stats()
        for tt in range(NT):
            k = tt % 2
            self.op("dve", lambda e, tt=tt, k=k: e.tensor_scalar(out=self.xs[k], in0=self.h[:, tt, :],
                                                                 scalar1=self.rstd[:, tt:tt + 1], scalar2=None,
                                                                 op0=ALU.mult),
                    reads=[("h", tt), "rstd"], writes=self.XS_KEYS[k])
            for g in range(2):
                b = self.galloc()
                for q in range(4):
                    kc = 4 * g + q
                    self.op("pe", lambda e, b=b, q=q, kc=kc, k=k: e.transpose(
                        out=self.ps[b][:, q * 128:(q + 1) * 128], in_=self.xs[k][:, kc * 128:(kc + 1) * 128],
                        identity=self.identf[:]),
                        reads=self.XS_KEYS[k] + ["identf"], writes=[("ps", b)], signal=(q == 3))
                self.op("dve", lambda e, b=b, g=g, tt=tt: e.tensor_tensor(
                    out=self.hnT[:, 4 * g:4 * g + 4, tt * 128:(tt + 1) * 128],
                    in0=self.ps[b][:, :].rearrange("p (k t) -> p k t", t=128),
                    in1=self.gT[:, gi, 4 * g:4 * g + 4].unsqueeze(2).to_broadcast([128, 4, 128]), op=ALU.mult),
                    reads=[("ps", b), "gT"], writes=[("hnT", tt)])

    def proj_T(self, wv, col0, dst, dkey):
        for tb in range(NB):
            b = self.galloc()
            for kc in range(KC):
                self.op("pe", lambda e, b=b, kc=kc, tb=tb: e.matmul(
                    self.ps[b][:, :], lhsT=wv[:, kc, col0:col0 + 128], rhs=self.hnT[:, kc, tb * 512:(tb + 1) * 512],
                    start=(kc == 0), stop=(kc == KC - 1)),
                    reads=[("hnT", tt) for tt in range(4 * tb, 4 * tb + 4)] + self.wreads, writes=[("ps", b)],
                    signal=(kc == KC - 1))
            self.op("act", lambda e, b=b, tb=tb: e.copy(out=dst[0][0:64, tb * 512:(tb + 1) * 512], in_=self.ps[b][0:64, :]),
                    reads=[("ps", b)], writes=[(dkey, 0, tb), ("psx", b)])
            self.op("dve", lambda e, b=b, tb=tb: e.tensor_copy(out=dst[1][0:64, tb * 512:(tb + 1) * 512],
                                                               in_=self.ps[b][64:128, :]),
                    reads=[("ps", b)], writes=[(dkey, 1, tb), ("psx", b)])

    def proj_V(self, wv, col0):
        for g in range(4):
            b = self.galloc()
            for tl in range(4):
                tt = 4 * g + tl
                for kc in range(KC):
                    self.op("pe", lambda e, b=b, tl=tl, tt=tt, kc=kc: e.matmul(
                        self.ps[b][:, tl * 128:(tl + 1) * 128], lhsT=self.hnT[:, kc, tt * 128:(tt + 1) * 128],
                        rhs=wv[:, kc, col0:col0 + 128], start=(kc == 0), stop=(kc == KC - 1), skip_group_check=True),
                        reads=[("hnT", tt)] + self.wreads, writes=[("ps", b)], signal=(kc == KC - 1 and tl == 3))
            pv = self.ps[b][:, :].rearrange("p (t f) -> p t f", f=128)
            self.op("act", lambda e, pv=pv, g=g: e.copy(out=self.Vp[:, 4 * g:4 * g + 4, 0:64], in_=pv[:, :, 0:64]),
                    reads=[("ps", b)], writes=[("Vp", g, 0), ("psx", b)])
            self.op("dve", lambda e, pv=pv, g=g: e.tensor_copy(out=self.Vp[:, 4 * g:4 * g + 4, 128:192], in_=pv[:, :, 64:128]),
                    reads=[("ps", b)], writes=[("Vp", g, 1), ("psx", b)])

    def moba_gate(self, pair):
        for i in range(2):
            hd = 2 * pair + i
            self.op("sp", lambda e, i=i, hd=hd: e.dma_start(out=self.QT[i][64:72, :], in_=self.augq_d[hd]),
                    writes=[("QTaug", i)], dsem=f"qa{i}")
            self.op("dve", lambda e, i=i: e.tensor_reduce(out=self.kmf[:, i, :],
                                                          in_=self.KT[i][0:64, :].rearrange("p (n k) -> p n k", k=256),
                                                          axis=AX.X, op=ALU.add),
                    reads=[("KT", i, tb) for tb in range(NB)], writes=[("kmf", i)])
            self.op("dve", lambda e, i=i: e.tensor_scalar(out=self.kmT[:, i, :], in0=self.kmf[:, i, :], scalar1=1.0 / 256,
                                                          scalar2=None, op0=ALU.mult),
                    reads=[("kmf", i)], writes=[("kmT", i)])
        gb = self.galloc()
        for qq in range(8):
            for i in range(2):
                grp = qq * 2 + i
                qt = 8 + qq
                self.op("pe", lambda e, i=i, grp=grp, qt=qt: e.matmul(
                    self.ps[gb][:, grp * 8:(grp + 1) * 8], lhsT=self.QT[i][0:64, qt * 128:(qt + 1) * 128],
                    rhs=self.kmT[:, i, :], start=True, stop=True, skip_group_check=True),
                    reads=[("QT", i, qt // 4), ("kmT", i)], writes=[("ps", gb)], signal=(grp == 15))
        self.op("dve", lambda e: e.tensor_tensor(out=self.g2[:], in0=self.ps[gb][:, 0:128], in1=self.gmask[:], op=ALU.add),
                reads=[("ps", gb), "gmask"], writes=["g2"])
        for grp in range(16):
            self.op("dve", lambda e, grp=grp: e.max(out=self.top8[:, grp * 8:(grp + 1) * 8], in_=self.g2[:, grp * 8:(grp + 1) * 8]),
                    reads=["g2"], writes=["top8"])
        g3 = lambda t: t[:, :].rearrange("p (g n) -> p g n", n=8)
        self.op("dve", lambda e: e.tensor_tensor(out=g3(self.selt), in0=g3(self.g2),
                                                 in1=g3(self.top8)[:, :, 2:3].to_broadcast([128, 16, 8]), op=ALU.is_ge),
                reads=["g2", "top8"], writes=["selt"])
        self.op("dve", lambda e: e.tensor_scalar(out=self.selt[:], in0=self.selt[:], scalar1=1.0, scalar2=-NEG,
                                                 op0=ALU.subtract, op1=ALU.mult), reads=["selt"], writes=["selt"])
        self.op("dve", lambda e: e.tensor_tensor(out=self.augtok[:, 64:192], in0=self.selt[:], in1=self.gvalid[:], op=ALU.mult),
                reads=["selt", "gvalid"], writes=["augtok"])

    def moba_gate2(self, pair):
        for i in range(2):
            for jj in range(2):
                b = self.galloc()
                for q in range(4):
                    grp = (4 * jj + q) * 2 + i
                    self.op("pe", lambda e, b=b, q=q, grp=grp: e.transpose(
                        out=self.ps[b][0:72, q * 128:(q + 1) * 128], in_=self.augtok[:, 8 * grp:8 * grp + 72],
                        identity=self.identf[:]),
                        reads=["augtok", "identf", "augpad"], writes=[("ps", b)], signal=(q == 3))
                c0 = 1024 + 512 * jj
                self.op("dve", lambda e, b=b, i=i, c0=c0: e.tensor_tensor(
                    out=self.QT[i][64:72, c0:c0 + 512], in0=self.ps[b][64:72, :], in1=self.QT[i][64:72, c0:c0 + 512],
                    op=ALU.add), reads=[("ps", b), ("QTaug", i)], writes=[("QTaug", i)])

    def attention(self, pair, K, btab, bkey, groups=None, hook=None):
        if groups is None:
            groups = [(i, j) for i in range(2) for j in range(NB)]
        blocks = [(i, j, kt) for (i, j) in groups for kt in range(4 * j + 4)]
        nblk = len(blocks)
        hook_idx = None
        if hook is not None:
            hook_idx = min(n for n, (i, j, kt) in enumerate(blocks) if j >= 2)

        def qk(n):
            i, j, kt = blocks[n]
            sbk = n % 2
            r = kt - 4 * j
            off = 128 * r if r > 0 else 0
            N = 512 - off
            diag = r >= 0
            rd = [("QT", i, j), ("QTaug", i), ("KT", i, kt // 4), f"KTaug{i}"]
            self.op("pe", lambda e: e.matmul(self.ps[sbk][:, 0:N], lhsT=self.KT[i][0:K, kt * 128:(kt + 1) * 128],
                                            rhs=self.QT[i][0:K, 512 * j + off:512 * j + 512], start=True, stop=(not diag)),
                    reads=rd, writes=[("ps", sbk)], signal=(not diag))
            if diag:
                self.op("pe", lambda e: e.matmul(self.ps[sbk][:, 0:128], lhsT=self.identb[:], rhs=self.trimask[:],
                                                start=False, stop=True),
                        reads=["identb", "trimask"], writes=[("ps", sbk)], signal=True)

        def exp_pv(n, gidx):
            i, j, kt = blocks[n]
            sbk = n % 2
            pb = n % 4
            ab = 2 + gidx % 2
            r = kt - 4 * j
            off = 128 * r if r > 0 else 0
            N = 512 - off
            hd = 2 * pair + i
            self.op("act", lambda e: e.activation(out=self.PT[:, pb, 0:N], in_=self.ps[sbk][:, 0:N], func=AF.Exp,
                                                  bias=btab[:, kt, hd:hd + 1], scale=SCALE),
                    reads=[("ps", sbk), bkey], writes=[("PT", pb)])
            last = (kt == 4 * j + 3)
            self.op("pe", lambda e: e.matmul(self.ps[ab][:, off:512], lhsT=self.Vp[:, kt, 64 * i:64 * i + 128],
                                            rhs=self.PT[:, pb, 0:N], start=(kt == 0), stop=last),
                    reads=[("PT", pb), ("Vp", kt // 4, i), "Vones"], writes=[("ps", ab)], signal=last)
            if last:
                x = gidx % 2
                num = slice(0, 64) if i == 0 else slice(64, 128)
                den = slice(64, 128) if i == 0 else slice(0, 64)
                self.op("dve", lambda e: e.reciprocal(out=self.rc[num, x, :], in_=self.ps[ab][den, :]),
                        reads=[("ps", ab)], writes=[("rc", x)])
                self.op("dve", lambda e: e.tensor_tensor(out=self.mixT[num, pair, 512 * j:512 * j + 512],
                                                         in0=self.ps[ab][num, :], in1=self.rc[num, x, :], op=ALU.mult),
                        reads=[("ps", ab), ("rc", x)], writes=[("mixT", pair, i, j)])

        gidx = 0
        if hook_idx == 0:
            hook()
        qk(0)
        for n in range(nblk):
            if n + 1 < nblk:
                if n + 1 == hook_idx:
                    hook()
                qk(n + 1)
            exp_pv(n, gidx)
            i, j, kt = blocks[n]
            if kt == 4 * j + 3:
                gidx += 1

    def wo(self):
        for hf in range(2):
            s, wv = self.wget("wo")
            self.wreads = [("ring", s, 0)]
            for tt in range(NT):
                b = self.galloc()
                for kc in range(KC):
                    self.op("pe", lambda e, b=b, kc=kc, tt=tt: e.matmul(
                        self.ps[b][:, :], lhsT=self.mixT[:, kc, tt * 128:(tt + 1) * 128], rhs=wv[:, kc, :],
                        start=(kc == 0), stop=(kc == KC - 1)),
                        reads=[("mixT", kc, i, tt // 4) for i in range(2)] + self.wreads, writes=[("ps", b)],
                        signal=(kc == KC - 1))
                self.op("dve", lambda e, b=b, tt=tt, hf=hf: e.tensor_tensor(
                    out=self.h[:, tt, hf * 512:(hf + 1) * 512], in0=self.ps[b][:, :],
                    in1=self.h[:, tt, hf * 512:(hf + 1) * 512], op=ALU.add),
                    reads=[("ps", b), ("h", tt)], writes=[("h", tt)])
            self.wdone()

    def mlp_up(self, buf):
        s, wv = self.wget("up")
        wr = [("ring", s, 0)]
        for fs in range(4):
            for tb in range(NB):
                b = self.galloc()
                for kc in range(KC):
                    self.op("pe", lambda e, b=b, kc=kc, fs=fs, tb=tb: e.matmul(
                        self.ps[b][:, :], lhsT=wv[:, kc, fs * 128:(fs + 1) * 128], rhs=self.hnT[:, kc, tb * 512:(tb + 1) * 512],
                        start=(kc == 0), stop=(kc == KC - 1)),
                        reads=[("hnT", tt) for tt in range(4 * tb, 4 * tb + 4)] + wr, writes=[("ps", b)],
                        signal=(kc == KC - 1))
                x = (fs * NB + tb) % 2
                self.op("act", lambda e, b=b, x=x: e.activation(out=self.rc[:, x, :], in_=self.ps[b][:, :], func=AF.Relu),
                        reads=[("ps", b)], writes=[("rc", x)])
                self.op("dve", lambda e, x=x, fs=fs, tb=tb: e.tensor_tensor(
                    out=self.uT[buf][:, fs, tb * 512:(tb + 1) * 512], in0=self.rc[:, x, :], in1=self.rc[:, x, :], op=ALU.mult),
                    reads=[("rc", x)], writes=[("uT", buf, fs, tb)])
        self.wdone()

    def mlp_down(self, buf):
        s, wv = self.wget("down")
        wr = [("ring", s, 0)]
        for tt in range(NT):
            for hf in range(2):
                b = self.galloc()
                for fs in range(4):
                    self.op("pe", lambda e, b=b, fs=fs, tt=tt, hf=hf: e.matmul(
                        self.ps[b][:, :], lhsT=self.uT[buf][:, fs, tt * 128:(tt + 1) * 128],
                        rhs=wv[:, fs, hf * 512:(hf + 1) * 512], start=(fs == 0), stop=(fs == 3)),
                        reads=[("uT", buf, fs, tt // 4)] + wr, writes=[("ps", b)], signal=(fs == 3))
                self.op("dve", lambda e, b=b, tt=tt, hf=hf: e.tensor_tensor(
                    out=self.h[:, tt, hf * 512:(hf + 1) * 512], in0=self.ps[b][:, :],
                    in1=self.h[:, tt, hf * 512:(hf + 1) * 512], op=ALU.add),
                    reads=[("ps", b), ("h", tt)], writes=[("h", tt)])
        self.wdone()

    def mlp(self):
        self.mlp_up(0)
        for c in range(8):
            if c + 1 < 8:
                self.mlp_up((c + 1) % 2)
            self.mlp_down(c % 2)

    def shared_kv(self):
        self.norm(8)
        for pair in range(8):
            s, wv = self.wget("kv")
            self.wreads = [("ring", s, 0)]
            self.proj_T(wv[0], 0, self.KT, "KT")
            self.wreads = [("ring", s, 1)]
            self.proj_V(wv[1], 0)
            self.wdone()
            for i in range(2):
                hd = 2 * pair + i
                self.op("sp", lambda e, i=i, hd=hd: e.dma_start(out=self.Ks[hd][0:64, :], in_=self.KT[i][0:64, :]),
                        reads=[("KT", i, tb) for tb in range(NB)], writes=[("Ks", hd)], dsem=f"ks{i}")
                self.op("sp", lambda e, hd=hd: e.dma_start(out=self.Ks[hd][64:65, :], in_=self.onesr[:]),
                        reads=["onesr"], writes=[("Ks1", hd)], dsem="ks1")
            self.op("sp", lambda e, pair=pair: e.dma_start(out=self.Vs[pair], in_=self.Vp[:, :, :].rearrange("p t f -> p (t f)")),
                    reads=[("Vp", g, i) for g in range(4) for i in range(2)] + ["Vones"], writes=[("Vs", pair)], dsem="vs")
        for hd in range(H):
            self.res[("Ks1", hd)][0] = ("ks1", self.dcount["ks1"])
        s, wv = self.wget("wf")
        ef = self.bigf[0:H, 0:S]
        spv = self.bigf[0:H, S:2 * S]
        C = self.bigf[0:H, 2 * S:3 * S]
        onesf = self.bigf[0:H, 3 * S:4 * S]
        caug = self.QTall[0:H, 0:S]
        BK = self.BIG_KEYS
        self.op("dve", lambda e: e.memset(onesf, 1.0), writes=BK)
        for tb in range(NB):
            b = self.galloc()
            for kc in range(KC):
                self.op("pe", lambda e, b=b, kc=kc, tb=tb: e.matmul(
                    self.ps[b][0:H, :], lhsT=wv[:, kc, :], rhs=self.hnT[:, kc, tb * 512:(tb + 1) * 512],
                    start=(kc == 0), stop=(kc == KC - 1)),
                    reads=[("hnT", tt) for tt in range(4 * tb, 4 * tb + 4)] + [("ring", s, 0)], writes=[("ps", b)],
                    signal=(kc == KC - 1))
            self.op("act", lambda e, b=b, tb=tb: e.activation(out=ef[:, tb * 512:(tb + 1) * 512], in_=self.ps[b][0:H, :],
                                                              func=AF.Exp, bias=self.nbf[:], scale=-1.0),
                    reads=[("ps", b), "nbf"], writes=BK)
        self.wdone()
        self.op("act", lambda e: e.activation(out=spv, in_=ef, func=AF.Ln, bias=1.0, scale=1.0), reads=BK, writes=BK)
        self.op("dve", lambda e: e.tensor_tensor_scan(out=C, data0=onesf, data1=spv, initial=0.0, op0=ALU.mult, op1=ALU.add),
                reads=BK, writes=BK)
        self.op("dve", lambda e: e.tensor_scalar(out=caug, in0=C, scalar1=-1.0 / SCALE, scalar2=None, op0=ALU.mult),
                reads=BK, writes=self.XS_KEYS[0])
        self.op("sp", lambda e: e.dma_start(out=self.Cs, in_=caug), reads=self.XS_KEYS[0], writes=["Cs"], dsem="cs")
        b = self.galloc()
        for kt in range(NT):
            self.op("pe", lambda e, b=b, kt=kt: e.transpose(out=self.ps[b][:, kt * H:(kt + 1) * H],
                                                           in_=C[:, kt * 128:(kt + 1) * 128], identity=self.identf[0:H, 0:H]),
                    reads=BK + ["identf"], writes=[("ps", b)], signal=(kt == NT - 1))
        self.op("dve", lambda e, b=b: e.tensor_copy(out=self.cbias[:, :, :],
                                                    in_=self.ps[b][:, 0:NT * H].rearrange("p (k h) -> p k h", h=H)),
                reads=[("ps", b)], writes=["cbias"])

    def fox_load(self, pair):
        for i in range(2):
            hd = 2 * pair + i
            self.op("sp", lambda e, i=i, hd=hd: e.dma_start(out=self.KT[i][0:65, :], in_=self.Ks[hd]),
                    reads=[("Ks", hd), ("Ks1", hd)], writes=[("KT", i, tb) for tb in range(NB)], dsem=f"kt{i}")
            self.op("sp", lambda e, i=i, hd=hd: e.dma_start(out=self.QT[i][64:65, :], in_=self.Cs[hd:hd + 1, :]),
                    reads=["Cs"], writes=[("QTaug", i)], dsem=f"qa{i}")
        self.op("sp", lambda e, pair=pair: e.dma_start(out=self.Vp[:, :, :].rearrange("p t f -> p (t f)"), in_=self.Vs[pair]),
                reads=[("Vs", pair)], writes=[("Vp", g, i) for g in range(4) for i in range(2)], dsem="vp")

    def final(self):
        self.stats()
        for tt in range(NT):
            k = tt % 2
            self.op("dve", lambda e, tt=tt, k=k: e.scalar_tensor_tensor(
                out=self.xs[k], in0=self.h[:, tt, :], scalar=self.rstd[:, tt:tt + 1], in1=self.gfin[:],
                op0=ALU.mult, op1=ALU.mult), reads=[("h", tt), "rstd", "gfin"], writes=self.XS_KEYS[k])
            self.op("sp", lambda e, tt=tt, k=k: e.dma_start(out=self.out[tt * 128:(tt + 1) * 128, :], in_=self.xs[k]),
                    reads=self.XS_KEYS[k], writes=[("out", tt)], dsem=f"o{k}")
        self.finish(["o0", "o1"])

    def dump_h(self):
        for tt in range(NT):
            self.op("sp", lambda e, tt=tt: e.dma_start(out=self.out[tt * 128:(tt + 1) * 128, :], in_=self.h[:, tt, :]),
                    reads=[("h", tt)], writes=[("out", tt)], dsem="o0")
        self.finish(["o0"])

    def dump_dbg(self):
        nc = self.nc
        dv = nc.dram_tensor("dbg_v", [128, NT * 192], BF16, kind="ExternalOutput").ap()
        dq = nc.dram_tensor("dbg_q", [128, 2 * S], BF16, kind="ExternalOutput").ap()
        dk = nc.dram_tensor("dbg_k", [128, 2 * S], BF16, kind="ExternalOutput").ap()
        dr = nc.dram_tensor("dbg_r", [128, NSLOT * SLOT], BF16, kind="ExternalOutput").ap()
        self.op("sp", lambda e: e.dma_start(out=dv, in_=self.Vp[:, :, :].rearrange("p t f -> p (t f)")),
                reads=[("Vp", g, i) for g in range(4) for i in range(2)], dsem="o0")
        self.op("sp", lambda e: e.dma_start(out=dq, in_=self.QTall[:, :]),
                reads=self.XS_KEYS[0] + self.XS_KEYS[1], dsem="o0")
        self.op("sp", lambda e: e.dma_start(out=dk, in_=self.KTall[:, :]),
                reads=[("KT", i, tb) for i in range(2) for tb in range(NB)], dsem="o0")
        self.op("sp", lambda e: e.dma_start(out=dr, in_=self.ring[:, :]),
                reads=[("ring", s_, t) for s_ in range(NSLOT) for t in range(3)], dsem="o0")
        self.dump_h()

    def finish(self, keys):
        sp = self.engs["sp"]
        for k in list(keys) + [k for k in self.dcount if k not in keys]:
            if self.dcount[k] > 0:
                sp.h.wait_ge(self.sems[k], self.dcount[k])

    def build(self):
        with self.es:
            self.declare()
            self.setup()
            self.build_wlist()
            self._body()
        return self.nc

    def _body(self):
        for L in range(DEPTH):
            if L == N_A:
                self.shared_kv()
                if self.stop == "kv":
                    return self.dump_h()
            if self.stop == "setup":
                return self.dump_h()
            self.norm(L)
            if self.stop == "norm":
                return self.dump_h()
            for pair in (self.pair_order if L < N_A else range(8)):
                if L < N_A:
                    s, wv = self.wget("qkv")
                    self.wreads = [("ring", s, 0)]
                    self.proj_T(wv[0], 0, self.QT, "QT")
                    self.wreads = [("ring", s, 1)]
                    self.proj_T(wv[1], 0, self.KT, "KT")
                    self.wreads = [("ring", s, 2)]
                    self.proj_V(wv[2], 0)
                    self.wdone()
                    if self.stop == "proj":
                        return self.dump_h()
                    self.moba_gate(pair)
                    grp = [(i, j) for j in (0, 1) for i in range(2)] + [(i, j) for j in (2, 3) for i in range(2)]
                    self.attention(pair, 72, self.abias, "abias", groups=grp,
                                   hook=lambda pair=pair: self.moba_gate2(pair))
                    if self.stop == "att":
                        return self.dump_h()
                else:
                    s, wv = self.wget("q")
                    self.wreads = [("ring", s, 0)]
                    self.fox_load(pair)
                    self.proj_T(wv[0], 0, self.QT, "QT")
                    self.wdone()
                    self.attention(pair, 65, self.cbias, "cbias")
            self.wo()
            if self.stop == f"attn{L}":
                return self.dump_h()
            self.norm(4 + L)
            self.mlp()
            if self.stop == f"mlp{L}":
                return self.dump_h()
        self.final()


def _consts():
    bf = ml_dtypes.bfloat16
    slopes = (2.0 ** (-8.0 * np.arange(1, H + 1) / H)).astype(np.float32)
    p = np.arange(128)
    c = {}
    c["identf"] = np.eye(128, dtype=np.float32)
    c["identb"] = np.eye(128, dtype=np.float32).astype(bf)
    c["trimask"] = np.where(p[:, None] > p[None, :], NEG, 0.0).astype(np.float32).astype(bf)
    c["kblock"] = (np.arange(S)[None, :] // 256 == np.arange(8)[:, None]).astype(np.float32).astype(bf)
    pos = (128 * np.arange(NT)[None, :] + p[:, None]).astype(np.float32)
    c["abias"] = np.ascontiguousarray((pos[:, :, None] * slopes[None, None, :]).astype(np.float32))
    aq = (-(1.0 / SCALE) * slopes[:, None] * np.arange(S, dtype=np.float32)[None, :]).astype(np.float32)
    c["augq"] = np.ascontiguousarray(np.broadcast_to(aq[:, None, :], (H, 8, S))).astype(bf)
    gm = np.zeros((128, 8, 2, 8), np.float32)
    gv = np.zeros((128, 8, 2, 8), np.float32)
    for qq in range(8):
        b = (8 + qq) // 2
        gm[:, qq, :, b:] = -1e30
        gv[:, qq, :, :b] = 1.0
    c["gmask"] = gm.reshape(128, 128)
    c["gvalid"] = gv.reshape(128, 128)
    return c


_CACHE = {}


def _get_nc(stop=None):
    if stop not in _CACHE:
        _CACHE[stop] = B(stop).build()
    return _CACHE[stop]


def _in_maps(inputs, cores):
    f = lambda a: np.ascontiguousarray(np.asarray(a, dtype=np.float32))
    gt = np.concatenate([f(inputs["attn_norm"]), f(inputs["mlp_norm"]), f(inputs["kv_norm"])[None, :]], axis=0)
    gtab = np.ascontiguousarray(gt.reshape(9, KC, 128).transpose(2, 0, 1))
    def pairwise(w, nt):
        lead = w.shape[:-2]
        w = w.reshape(lead + (KC, 128, nt, 8, 128))
        nl = len(lead)
        perm = tuple(range(nl)) + (nl + 3, nl + 1, nl + 2, nl + 0, nl + 4)
        return np.ascontiguousarray(w.transpose(perm)).reshape(lead + (8, 128, nt * KC * 128))
    shared = {
        "wqkv_r": pairwise(f(inputs["moba_w_qkv"]), 3), "wq_r": pairwise(f(inputs["fox_w_q"]), 1), "w_o": f(inputs["w_o"]),
        "wkv_r": pairwise(f(inputs["w_kv"]), 2),
        "wf_r": np.ascontiguousarray(f(inputs["w_f"]).reshape(KC, 128, H).transpose(1, 0, 2)).reshape(128, KC * H),
        "b_f": f(inputs["b_f"]).reshape(H, 1),
        "w_up": f(inputs["w_up"]), "w_down": f(inputs["w_down"]), "final_norm": f(inputs["final_norm"]),
        "gtab": gtab,
    }
    shared.update(_consts())
    x = f(inputs["x"])
    return [dict(shared, x=np.ascontiguousarray(x[b])) for b in cores]


def kernel(**inputs):
    nc = _get_nc(None)
    maps = _in_maps(inputs, list(range(8)))
    res = run_bass_kernel_spmd(nc, maps, core_ids=list(range(8)))
    return np.stack([np.asarray(r["out"], dtype=np.float32) for r in res.results], axis=0)
```

```python
import contextlib
import numpy as np
import ml_dtypes
import concourse.bass as bass
import concourse.mybir as mybir
from concourse.bass_utils import run_bass_kernel_spmd

F32 = mybir.dt.float32
BF16 = mybir.dt.bfloat16
AF = mybir.ActivationFunctionType
ALU = mybir.AluOpType
AX = mybir.AxisListType

S = 2048
D = 1024
H = 16
DH = 64
FF = 4096
NT = 16
NB = 4
KC = 8
NSLOT = 4
SLOT = 4096
NEG = -30000.0
EPS = 1e-6
SCALE = 0.125
N_A = 2
DEPTH = 4


class Eng:
    def __init__(self, name, h):
        self.name = name
        self.h = h
        self.nsig = 0
        self.seen = {}


class B:
    def __init__(self, stop=None):
        self.stop = stop
        import os as _os
        self.pair_order = list(range(8))[::-1] if _os.environ.get("KREV") else list(range(8))
        self.nc = bass.Bass("TRN2", target_bir_lowering=False)
        self.es = contextlib.ExitStack()
        nc = self.nc
        self.engs = {"pe": Eng("pe", nc.tensor), "act": Eng("act", nc.scalar), "dve": Eng("dve", nc.vector),
                     "pool": Eng("pool", nc.gpsimd), "sp": Eng("sp", nc.sync)}
        self.sems = {}
        self.dcount = {}
        for n in self.engs:
            self.sems[n] = self.es.enter_context(nc.semaphore("s_" + n))
        self.res = {}
        self.const_keys = set()
        self.gbank = 0
        self.G_FULL = [0, 1, 2, 3, 4, 5, 6]
        self.G_ATTN = [5, 6]
        self.gset = self.G_FULL

    def dsem(self, name):
        if name not in self.sems:
            self.sems[name] = self.es.enter_context(self.nc.semaphore("d_" + name))
            self.dcount[name] = 0
        return name

    def sb(self, name, shape, dt):
        return self.es.enter_context(self.nc.sbuf_tensor(name, shape, dt))

    def _wait(self, eng, ev):
        key, val = ev
        if eng.seen.get(key, 0) >= val:
            return
        eng.h.wait_ge(self.sems[key], val)
        eng.seen[key] = val

    def op(self, en, fn, reads=(), writes=(), signal=True, dsem=None):
        eng = self.engs[en]
        evs = []
        for r in reads:
            st = self.res.get(r)
            if st is not None and st[0] is not None:
                evs.append(st[0])
        for w in writes:
            st = self.res.get(w)
            if st is not None:
                if st[0] is not None:
                    evs.append(st[0])
                evs.extend(st[1].items())
        for ev in evs:
            if ev[0] == "pe" and en == "pe":
                continue
            if ev[0] in self.engs:
                assert self.engs[ev[0]].nsig >= ev[1], ("unsignaled dependency", en, ev, reads, writes)
            self._wait(eng, ev)
        ins = fn(eng.h)
        if dsem is not None:
            self.dsem(dsem)
            ins.then_inc(self.sems[dsem], 16)
            self.dcount[dsem] += 16
            ev = (dsem, self.dcount[dsem])
        elif signal:
            ins.then_inc(self.sems[en], 1)
            eng.nsig += 1
            ev = (en, eng.nsig)
        else:
            ev = (en, eng.nsig + 1)
        for w in writes:
            self.res[w] = [ev, {}]
        for r in reads:
            if r in self.const_keys:
                continue
            st = self.res.setdefault(r, [None, {}])
            if st[1].get(ev[0], 0) < ev[1]:
                st[1][ev[0]] = ev[1]
        return ev

    def galloc(self):
        b = self.gset[self.gbank % len(self.gset)]
        self.gbank += 1
        return b

    def declare(self):
        nc = self.nc
        di = lambda n, s, d=F32: nc.dram_tensor(n, s, d, kind="ExternalInput").ap()
        self.x = di("x", [S, D])
        self.w_qkv = di("wqkv_r", [N_A, 8, 128, 3 * KC * 128])
        self.w_q = di("wq_r", [DEPTH - N_A, 8, 128, KC * 128])
        self.w_o = di("w_o", [DEPTH, D, D])
        self.w_kv = di("wkv_r", [8, 128, 2 * KC * 128])
        self.w_f = di("wf_r", [128, KC * H])
        self.b_f = di("b_f", [H, 1])
        self.w_up = di("w_up", [DEPTH, D, FF])
        self.w_down = di("w_down", [DEPTH, FF, D])
        self.final_norm = di("final_norm", [D])
        self.gtab_d = di("gtab", [128, 9, KC])
        self.identf_d = di("identf", [128, 128])
        self.identb_d = di("identb", [128, 128], BF16)
        self.trimask_d = di("trimask", [128, 128], BF16)
        self.kblock_d = di("kblock", [8, S], BF16)
        self.abias_d = di("abias", [128, NT, H])
        self.augq_d = di("augq", [H, 8, S], BF16)
        self.gmask_d = di("gmask", [128, 128])
        self.gvalid_d = di("gvalid", [128, 128])
        self.Ks = nc.dram_tensor("Ks", [H, 65, S], BF16, kind="Internal").ap()
        self.Vs = nc.dram_tensor("Vs", [H // 2, 128, NT * 192], BF16, kind="Internal").ap()
        self.Cs = nc.dram_tensor("Cs", [H, S], BF16, kind="Internal").ap()
        self.out = nc.dram_tensor("out", [S, D], F32, kind="ExternalOutput").ap()

        sb = self.sb
        self.h = sb("h", [128, NT, D], F32)
        self.hnT = sb("hnT", [128, KC, S], BF16)
        self.big = sb("big", [128, 16384], BF16)
        self.ring = sb("ring", [128, NSLOT * SLOT], BF16)
        self.QTall = sb("QTall", [128, 2 * S], BF16)
        self.KTall = sb("KTall", [128, 2 * S], BF16)
        self.Vp = sb("Vp", [128, NT, 192], BF16)
        self.PT = sb("PT", [128, 4, 512], BF16)
        self.rc = sb("rc", [128, 2, 512], F32)
        self.gfin = sb("gfin", [128, D], F32)
        self.gT = sb("gT", [128, 9, KC], F32)
        self.identf = sb("identf_s", [128, 128], F32)
        self.identb = sb("identb_s", [128, 128], BF16)
        self.trimask = sb("trimask_s", [128, 128], BF16)
        self.abias = sb("abias_s", [128, NT, H], F32)
        self.cbias = sb("cbias_s", [128, NT, H], F32)
        self.gmask = sb("gmask_s", [128, 128], F32)
        self.gvalid = sb("gvalid_s", [128, 128], F32)
        self.ss = sb("ss", [128, NT], F32)
        self.rt = sb("rt", [128, NT], F32)
        self.rstd = sb("rstd", [128, NT], F32)
        self.bft = sb("bft", [H, 1], F32)
        self.nbf = sb("nbf", [H, 1], F32)
        self.kmf = sb("kmf", [64, 2, 8], F32)
        self.kmT = sb("kmT", [64, 2, 8], BF16)
        self.g2 = sb("g2", [128, 128], F32)
        self.top8 = sb("top8", [128, 128], F32)
        self.selt = sb("selt", [128, 128], F32)
        self.augtok = sb("augtok", [128, 64 + 128], F32)
        self.onesr = sb("onesr", [1, S], BF16)
        self.ps = [self.es.enter_context(nc.psum_tensor(f"ps{b}", [128, 512], F32)) for b in range(8)]

        self.QT = [self.QTall[:, i * S:(i + 1) * S] for i in range(2)]
        self.KT = [self.KTall[:, i * S:(i + 1) * S] for i in range(2)]
        qf = self.QTall[:, :].bitcast(F32)
        self.xs = [qf[:, k * 1024:(k + 1) * 1024] for k in range(2)]
        self.mixT = self.big[:, :].rearrange("p (c t) -> p c t", t=S)
        self.uT = [self.big[:, b * 8192:(b + 1) * 8192].rearrange("p (f t) -> p f t", t=S) for b in range(2)]
        self.bigf = self.big[:, :].bitcast(F32)
        self.XS_KEYS = [[("QT", k, tb) for tb in range(NB)] + [("QTaug", k)] for k in range(2)]
        self.BIG_KEYS = ([("uT", b, fs, tb) for b in range(2) for fs in range(4) for tb in range(NB)]
                         + [("mixT", p, i, j) for p in range(8) for i in range(2) for j in range(NB)])

    def setup(self):
        c = self.dsem("c")
        consts = [
            (self.identf[:], self.identf_d, "identf"), (self.identb[:], self.identb_d, "identb"),
            (self.trimask[:], self.trimask_d, "trimask"), (self.abias[:], self.abias_d, "abias"),
            (self.gmask[:], self.gmask_d, "gmask"), (self.gvalid[:], self.gvalid_d, "gvalid"),
            (self.gT[:], self.gtab_d, "gT"), (self.gfin[:], self.final_norm.partition_broadcast(128), "gfin"),
            (self.bft[:], self.b_f, "bft"),
            (self.KT[0][64:72, :], self.kblock_d, "KTaug0"), (self.KT[1][64:72, :], self.kblock_d, "KTaug1"),
        ]
        for (o, i, k) in consts:
            self.op("sp", lambda e, o=o, i=i: e.dma_start(out=o, in_=i), writes=[k], dsem=c)
        tot = self.dcount[c]
        for (_, _, k) in consts:
            self.res[k] = [(c, tot), {}]
            if k != "bft":
                self.const_keys.add(k)
        xv = self.x.rearrange("(t p) f -> p t f", p=128)
        for g in range(4):
            self.op("sp", lambda e, g=g: e.dma_start(out=self.h[:, 4 * g:4 * g + 4, :], in_=xv[:, 4 * g:4 * g + 4, :]),
                    writes=[("h", tt) for tt in range(4 * g, 4 * g + 4)], dsem=f"x{g}")
        self.op("dve", lambda e: e.memset(self.Vp[:, :, 64:128], 1.0), writes=["Vones"])
        self.op("dve", lambda e: e.memset(self.augtok[:, 0:64], 0.0), writes=["augpad"])
        self.op("dve", lambda e: e.memset(self.onesr[:], 1.0), writes=["onesr"])
        self.op("dve", lambda e: e.tensor_scalar(out=self.nbf[:], in0=self.bft[:], scalar1=-1.0, scalar2=None,
                                                 op0=ALU.mult), reads=["bft"], writes=["nbf"])
        for k in ("Vones", "augpad", "onesr", "nbf"):
            self.const_keys.add(k)

    def ring_view(self, s, kc, f):
        return self.ring[:, s * SLOT:s * SLOT + kc * f].rearrange("p (k f) -> p k f", f=f)

    def build_wlist(self):
        wl = []
        rr = lambda ap: ap.rearrange("(k p) f -> p k f", p=128)

        def mlp(L):
            ups = [("up", [(0, (KC, 512), None, rr(self.w_up[L])[:, :, c * 512:(c + 1) * 512])]) for c in range(8)]
            dns = [("down", [(0, (4, D), None, rr(self.w_down[L][c * 512:(c + 1) * 512, :]))]) for c in range(8)]
            order = [ups[0]]
            for c in range(8):
                if c + 1 < 8:
                    order.append(ups[c + 1])
                order.append(dns[c])
            return order

        def wo(L):
            return [("wo", [(0, (KC, 512), None, rr(self.w_o[L])[:, :, hf * 512:(hf + 1) * 512])]) for hf in range(2)]

        for L in range(N_A):
            for p in self.pair_order:
                wl.append(("qkv", [((0, 1, 2), 3 * KC * 128, 3, self.w_qkv[L, p])]))
            wl += wo(L) + mlp(L)
        for p in range(8):
            wl.append(("kv", [((0, 1), 2 * KC * 128, 2, self.w_kv[p])]))
        wl.append(("wf", [((0,), KC * H, 0, self.w_f)]))
        for L in range(N_A, DEPTH):
            for p in range(8):
                wl.append(("q", [((0,), KC * 128, 1, self.w_q[L - N_A, p])]))
            wl += wo(L) + mlp(L)
        self.wl = wl
        self.wpos = 0
        self.wfill = 0
        for _ in range(NSLOT):
            self.fill_next()

    def fill_next(self):
        if self.wfill >= len(self.wl):
            return
        i = self.wfill
        self.wfill += 1
        s = i % NSLOT
        name, parts = self.wl[i]
        for (part, sz, nt, src) in parts:
            if nt is None:
                kc, f = sz
                dst = self.ring_view(s, kc, f)
            else:
                dst = self.ring[:, s * SLOT:s * SLOT + sz]
            keys = [("ring", s, t) for t in range(3)]
            self.op("pool", lambda e, dst=dst, src=src: e.dma_start(out=dst, in_=src), writes=keys, dsem=f"r{s}")

    def part_view(self, s, t):
        return self.ring[:, s * SLOT + t * 1024:s * SLOT + (t + 1) * 1024].rearrange("p (k c) -> p k c", c=128)

    def wget(self, name):
        i = self.wpos
        self.wpos += 1
        assert self.wl[i][0] == name, (self.wl[i][0], name)
        s = i % NSLOT
        parts = self.wl[i][1]
        nt = parts[0][2]
        if nt is not None:
            if nt == 0:
                return s, self.ring[:, s * SLOT:s * SLOT + KC * H].rearrange("p (k h) -> p k h", h=H)
            return s, [self.part_view(s, t) for t in range(nt)]
        kc, f = parts[0][1]
        return s, self.ring_view(s, kc, f)

    def wdone(self):
        self.fill_next()

    def stats(self):
        self.op("dve", lambda e: e.memset(self.ss[:], 0.0), writes=["ss"])
        for tt in range(NT):
            k = tt % 2
            self.op("act", lambda e, tt=tt, k=k: e.activation(out=self.xs[k], in_=self.h[:, tt, :], func=AF.Square,
                                                              accum_out=self.ss[:, tt:tt + 1]),
                    reads=[("h", tt), "ss"], writes=self.XS_KEYS[k] + [("ssc", tt)])
        self.op("act", lambda e: e.activation(out=self.rt[:], in_=self.ss[:], func=AF.Sqrt, bias=EPS, scale=1.0 / D),
                reads=[("ssc", tt) for tt in range(NT)] + ["ss"], writes=["rt"])
        self.op("dve", lambda e: e.reciprocal(out=self.rstd[:], in_=self.rt[:]), reads=["rt"], writes=["rstd"])

    def norm(self, gi):
        self.stats()
        for tt in range(NT):
            k = tt % 2
            self.op("dve", lambda e, tt=tt, k=k: e.tensor_scalar(out=self.xs[k], in0=self.h[:, tt, :],
                                                                 scalar1=self.rstd[:, tt:tt + 1], scalar2=None,
                                                                 op0=ALU.mult),
                    reads=[("h", tt), "rstd"], writes=self.XS_KEYS[k])
            for g in range(2):
                b = self.galloc()
                for q in range(4):
                    kc = 4 * g + q
                    self.op("pe", lambda e, b=b, q=q, kc=kc, k=k: e.transpose(
                        out=self.ps[b][:, q * 128:(q + 1) * 128], in_=self.xs[k][:, kc * 128:(kc + 1) * 128],
                        identity=self.identf[:]),
                        reads=self.XS_KEYS[k] + ["identf"], writes=[("ps", b)], signal=(q == 3))
                self.op("dve", lambda e, b=b, g=g, tt=tt: e.tensor_tensor(
                    out=self.hnT[:, 4 * g:4 * g + 4, tt * 128:(tt + 1) * 128],
                    in0=self.ps[b][:, :].rearrange("p (k t) -> p k t", t=128),
                    in1=self.gT[:, gi, 4 * g:4 * g + 4].unsqueeze(2).to_broadcast([128, 4, 128]), op=ALU.mult),
                    reads=[("ps", b), "gT"], writes=[("hnT", tt)])

    def proj_T(self, wv, col0, dst, dkey):
        for tb in range(NB):
            b = self.galloc()
            for kc in range(KC):
                self.op("pe", lambda e, b=b, kc=kc, tb=tb: e.matmul(
                    self.ps[b][:, :], lhsT=wv[:, kc, col0:col0 + 128], rhs=self.hnT[:, kc, tb * 512:(tb + 1) * 512],
                    start=(kc == 0), stop=(kc == KC - 1)),
                    reads=[("hnT", tt) for tt in range(4 * tb, 4 * tb + 4)] + self.wreads, writes=[("ps", b)],
                    signal=(kc == KC - 1))
            self.op("act", lambda e, b=b, tb=tb: e.copy(out=dst[0][0:64, tb * 512:(tb + 1) * 512], in_=self.ps[b][0:64, :]),
                    reads=[("ps", b)], writes=[(dkey, 0, tb), ("psx", b)])
            self.op("dve", lambda e, b=b, tb=tb: e.tensor_copy(out=dst[1][0:64, tb * 512:(tb + 1) * 512],
                                                               in_=self.ps[b][64:128, :]),
                    reads=[("ps", b)], writes=[(dkey, 1, tb), ("psx", b)])

    def proj_V(self, wv, col0):
        for g in range(4):
            b = self.galloc()
            for tl in range(4):
                tt = 4 * g + tl
                for kc in range(KC):
                    self.op("pe", lambda e, b=b, tl=tl, tt=tt, kc=kc: e.matmul(
                        self.ps[b][:, tl * 128:(tl + 1) * 128], lhsT=self.hnT[:, kc, tt * 128:(tt + 1) * 128],
                        rhs=wv[:, kc, col0:col0 + 128], start=(kc == 0), stop=(kc == KC - 1), skip_group_check=True),
                        reads=[("hnT", tt)] + self.wreads, writes=[("ps", b)], signal=(kc == KC - 1 and tl == 3))
            pv = self.ps[b][:, :].rearrange("p (t f) -> p t f", f=128)
            self.op("act", lambda e, pv=pv, g=g: e.copy(out=self.Vp[:, 4 * g:4 * g + 4, 0:64], in_=pv[:, :, 0:64]),
                    reads=[("ps", b)], writes=[("Vp", g, 0), ("psx", b)])
            self.op("dve", lambda e, pv=pv, g=g: e.tensor_copy(out=self.Vp[:, 4 * g:4 * g + 4, 128:192], in_=pv[:, :, 64:128]),
                    reads=[("ps", b)], writes=[("Vp", g, 1), ("psx", b)])

    def moba_gate(self, pair):
        for i in range(2):
            hd = 2 * pair + i
            self.op("sp", lambda e, i=i, hd=hd: e.dma_start(out=self.QT[i][64:72, :], in_=self.augq_d[hd]),
                    writes=[("QTaug", i)], dsem=f"qa{i}")
            self.op("dve", lambda e, i=i: e.tensor_reduce(out=self.kmf[:, i, :],
                                                          in_=self.KT[i][0:64, :].rearrange("p (n k) -> p n k", k=256),
                                                          axis=AX.X, op=ALU.add),
                    reads=[("KT", i, tb) for tb in range(NB)], writes=[("kmf", i)])
            self.op("dve", lambda e, i=i: e.tensor_scalar(out=self.kmT[:, i, :], in0=self.kmf[:, i, :], scalar1=1.0 / 256,
                                                          scalar2=None, op0=ALU.mult),
                    reads=[("kmf", i)], writes=[("kmT", i)])
        gb = self.galloc()
        for qq in range(8):
            for i in range(2):
                grp = qq * 2 + i
                qt = 8 + qq
                self.op("pe", lambda e, i=i, grp=grp, qt=qt: e.matmul(
                    self.ps[gb][:, grp * 8:(grp + 1) * 8], lhsT=self.QT[i][0:64, qt * 128:(qt + 1) * 128],
                    rhs=self.kmT[:, i, :], start=True, stop=True, skip_group_check=True),
                    reads=[("QT", i, qt // 4), ("kmT", i)], writes=[("ps", gb)], signal=(grp == 15))
        self.op("dve", lambda e: e.tensor_tensor(out=self.g2[:], in0=self.ps[gb][:, 0:128], in1=self.gmask[:], op=ALU.add),
                reads=[("ps", gb), "gmask"], writes=["g2"])
        for grp in range(16):
            self.op("dve", lambda e, grp=grp: e.max(out=self.top8[:, grp * 8:(grp + 1) * 8], in_=self.g2[:, grp * 8:(grp + 1) * 8]),
                    reads=["g2"], writes=["top8"])
        g3 = lambda t: t[:, :].rearrange("p (g n) -> p g n", n=8)
        self.op("dve", lambda e: e.tensor_tensor(out=g3(self.selt), in0=g3(self.g2),
                                                 in1=g3(self.top8)[:, :, 2:3].to_broadcast([128, 16, 8]), op=ALU.is_ge),
                reads=["g2", "top8"], writes=["selt"])
        self.op("dve", lambda e: e.tensor_scalar(out=self.selt[:], in0=self.selt[:], scalar1=1.0, scalar2=-NEG,
                                                 op0=ALU.subtract, op1=ALU.mult), reads=["selt"], writes=["selt"])
        self.op("dve", lambda e: e.tensor_tensor(out=self.augtok[:, 64:192], in0=self.selt[:], in1=self.gvalid[:], op=ALU.mult),
                reads=["selt", "gvalid"], writes=["augtok"])

    def moba_gate2(self, pair):
        for i in range(2):
            for jj in range(2):
                b = self.galloc()
                for q in range(4):
                    grp = (4 * jj + q) * 2 + i
                    self.op("pe", lambda e, b=b, q=q, grp=grp: e.transpose(
                        out=self.ps[b][0:72, q * 128:(q + 1) * 128], in_=self.augtok[:, 8 * grp:8 * grp + 72],
                        identity=self.identf[:]),
                        reads=["augtok", "identf", "augpad"], writes=[("ps", b)], signal=(q == 3))
                c0 = 1024 + 512 * jj
                self.op("dve", lambda e, b=b, i=i, c0=c0: e.tensor_tensor(
                    out=self.QT[i][64:72, c0:c0 + 512], in0=self.ps[b][64:72, :], in1=self.QT[i][64:72, c0:c0 + 512],
                    op=ALU.add), reads=[("ps", b), ("QTaug", i)], writes=[("QTaug", i)])

    def attention(self, pair, K, btab, bkey, groups=None, hook=None):
        if groups is None:
            groups = [(i, j) for i in range(2) for j in range(NB)]
        blocks = [(i, j, kt) for (i, j) in groups for kt in range(4 * j + 4)]
        nblk = len(blocks)
        hook_idx = None
        if hook is not None:
            hook_idx = min(n for n, (i, j, kt) in enumerate(blocks) if j >= 2)

        SB = [0, 1, 4]

        def qk(n):
            if n == hook_idx:
                hook()
            i, j, kt = blocks[n]
            sbk = SB[n % 3]
            r = kt - 4 * j
            off = 128 * r if r > 0 else 0
            N = 512 - off
            diag = r >= 0
            rd = [("QT", i, j), ("QTaug", i), ("KT", i, kt // 4), f"KTaug{i}"]
            self.op("pe", lambda e: e.matmul(self.ps[sbk][:, 0:N], lhsT=self.KT[i][0:K, kt * 128:(kt + 1) * 128],
                                            rhs=self.QT[i][0:K, 512 * j + off:512 * j + 512], start=True, stop=(not diag)),
                    reads=rd, writes=[("ps", sbk)], signal=(not diag))
            if diag:
                self.op("pe", lambda e: e.matmul(self.ps[sbk][:, 0:128], lhsT=self.identb[:], rhs=self.trimask[:],
                                                start=False, stop=True),
                        reads=["identb", "trimask"], writes=[("ps", sbk)], signal=True)

        def exp_pv(n, gidx):
            i, j, kt = blocks[n]
            sbk = SB[n % 3]
            pb = n % 4
            ab = 2 + gidx % 2
            r = kt - 4 * j
            off = 128 * r if r > 0 else 0
            N = 512 - off
            hd = 2 * pair + i
            self.op("act", lambda e: e.activation(out=self.PT[:, pb, 0:N], in_=self.ps[sbk][:, 0:N], func=AF.Exp,
                                                  bias=btab[:, kt, hd:hd + 1], scale=SCALE),
                    reads=[("ps", sbk), bkey], writes=[("PT", pb)])
            last = (kt == 4 * j + 3)
            self.op("pe", lambda e: e.matmul(self.ps[ab][:, off:512], lhsT=self.Vp[:, kt, 64 * i:64 * i + 128],
                                            rhs=self.PT[:, pb, 0:N], start=(kt == 0), stop=last),
                    reads=[("PT", pb), ("Vp", kt // 4, i), "Vones"], writes=[("ps", ab)], signal=last)
            self.op("pe", lambda e: e.matmul(self.ps[7][:, 0:256], lhsT=self.identb[:], rhs=self.hnT[:, 0, 0:256],
                                            start=True, stop=True), reads=["identb"], writes=[("ps", 7)], signal=False)
            if last:
                x = gidx % 2
                num = slice(0, 64) if i == 0 else slice(64, 128)
                den = slice(64, 128) if i == 0 else slice(0, 64)
                self.op("dve", lambda e: e.reciprocal(out=self.rc[num, x, :], in_=self.ps[ab][den, :]),
                        reads=[("ps", ab)], writes=[("rc", x)])
                self.op("dve", lambda e: e.tensor_tensor(out=self.mixT[num, pair, 512 * j:512 * j + 512],
                                                         in0=self.ps[ab][num, :], in1=self.rc[num, x, :], op=ALU.mult),
                        reads=[("ps", ab), ("rc", x)], writes=[("mixT", pair, i, j)])

        gidx = 0
        qk(0)
        qk(1)
        for n in range(nblk):
            if n + 2 < nblk:
                qk(n + 2)
            exp_pv(n, gidx)
            i, j, kt = blocks[n]
            if kt == 4 * j + 3:
                gidx += 1

    def wo(self):
        for hf in range(2):
            s, wv = self.wget("wo")
            self.wreads = [("ring", s, 0)]
            for tt in range(NT):
                b = self.galloc()
                for kc in range(KC):
                    self.op("pe", lambda e, b=b, kc=kc, tt=tt: e.matmul(
                        self.ps[b][:, :], lhsT=self.mixT[:, kc, tt * 128:(tt + 1) * 128], rhs=wv[:, kc, :],
                        start=(kc == 0), stop=(kc == KC - 1)),
                        reads=[("mixT", kc, i, tt // 4) for i in range(2)] + self.wreads, writes=[("ps", b)],
                        signal=(kc == KC - 1))
                self.op("dve", lambda e, b=b, tt=tt, hf=hf: e.tensor_tensor(
                    out=self.h[:, tt, hf * 512:(hf + 1) * 512], in0=self.ps[b][:, :],
                    in1=self.h[:, tt, hf * 512:(hf + 1) * 512], op=ALU.add),
                    reads=[("ps", b), ("h", tt)], writes=[("h", tt)])
            self.wdone()

    def mlp_up(self, buf):
        s, wv = self.wget("up")
        wr = [("ring", s, 0)]
        for fs in range(4):
            for tb in range(NB):
                b = self.galloc()
                for kc in range(KC):
                    self.op("pe", lambda e, b=b, kc=kc, fs=fs, tb=tb: e.matmul(
                        self.ps[b][:, :], lhsT=wv[:, kc, fs * 128:(fs + 1) * 128], rhs=self.hnT[:, kc, tb * 512:(tb + 1) * 512],
                        start=(kc == 0), stop=(kc == KC - 1)),
                        reads=[("hnT", tt) for tt in range(4 * tb, 4 * tb + 4)] + wr, writes=[("ps", b)],
                        signal=(kc == KC - 1))
                x = (fs * NB + tb) % 2
                self.op("act", lambda e, b=b, x=x: e.activation(out=self.rc[:, x, :], in_=self.ps[b][:, :], func=AF.Relu),
                        reads=[("ps", b)], writes=[("rc", x)])
                self.op("dve", lambda e, x=x, fs=fs, tb=tb: e.tensor_tensor(
                    out=self.uT[buf][:, fs, tb * 512:(tb + 1) * 512], in0=self.rc[:, x, :], in1=self.rc[:, x, :], op=ALU.mult),
                    reads=[("rc", x)], writes=[("uT", buf, fs, tb)])
        self.wdone()

    def mlp_down(self, buf):
        s, wv = self.wget("down")
        wr = [("ring", s, 0)]
        for tt in range(NT):
            for hf in range(2):
                b = self.galloc()
                for fs in range(4):
                    self.op("pe", lambda e, b=b, fs=fs, tt=tt, hf=hf: e.matmul(
                        self.ps[b][:, :], lhsT=self.uT[buf][:, fs, tt * 128:(tt + 1) * 128],
                        rhs=wv[:, fs, hf * 512:(hf + 1) * 512], start=(fs == 0), stop=(fs == 3)),
                        reads=[("uT", buf, fs, tt // 4)] + wr, writes=[("ps", b)], signal=(fs == 3))
                self.op("dve", lambda e, b=b, tt=tt, hf=hf: e.tensor_tensor(
                    out=self.h[:, tt, hf * 512:(hf + 1) * 512], in0=self.ps[b][:, :],
                    in1=self.h[:, tt, hf * 512:(hf + 1) * 512], op=ALU.add),
                    reads=[("ps", b), ("h", tt)], writes=[("h", tt)])
        self.wdone()

    def mlp(self):
        self.mlp_up(0)
        for c in range(8):
            if c + 1 < 8:
                self.mlp_up((c + 1) % 2)
            self.mlp_down(c % 2)

    def shared_kv(self):
        self.norm(8)
        for pair in range(8):
            s, wv = self.wget("kv")
            self.wreads = [("ring", s, 0)]
            self.proj_T(wv[0], 0, self.KT, "KT")
            self.wreads = [("ring", s, 1)]
            self.proj_V(wv[1], 0)
            self.wdone()
            for i in range(2):
                hd = 2 * pair + i
                self.op("sp", lambda e, i=i, hd=hd: e.dma_start(out=self.Ks[hd][0:64, :], in_=self.KT[i][0:64, :]),
                        reads=[("KT", i, tb) for tb in range(NB)], writes=[("Ks", hd)], dsem=f"ks{i}")
                self.op("sp", lambda e, hd=hd: e.dma_start(out=self.Ks[hd][64:65, :], in_=self.onesr[:]),
                        reads=["onesr"], writes=[("Ks1", hd)], dsem="kone")
            self.op("sp", lambda e, pair=pair: e.dma_start(out=self.Vs[pair], in_=self.Vp[:, :, :].rearrange("p t f -> p (t f)")),
                    reads=[("Vp", g, i) for g in range(4) for i in range(2)] + ["Vones"], writes=[("Vs", pair)], dsem="vs")
        for hd in range(H):
            self.res[("Ks1", hd)][0] = ("kone", self.dcount["kone"])
        s, wv = self.wget("wf")
        ef = self.bigf[0:H, 0:S]
        spv = self.bigf[0:H, S:2 * S]
        C = self.bigf[0:H, 2 * S:3 * S]
        onesf = self.bigf[0:H, 3 * S:4 * S]
        caug = self.QTall[0:H, 0:S]
        BK = self.BIG_KEYS
        self.op("dve", lambda e: e.memset(onesf, 1.0), writes=BK)
        for tb in range(NB):
            b = self.galloc()
            for kc in range(KC):
                self.op("pe", lambda e, b=b, kc=kc, tb=tb: e.matmul(
                    self.ps[b][0:H, :], lhsT=wv[:, kc, :], rhs=self.hnT[:, kc, tb * 512:(tb + 1) * 512],
                    start=(kc == 0), stop=(kc == KC - 1)),
                    reads=[("hnT", tt) for tt in range(4 * tb, 4 * tb + 4)] + [("ring", s, 0)], writes=[("ps", b)],
                    signal=(kc == KC - 1))
            self.op("act", lambda e, b=b, tb=tb: e.activation(out=ef[:, tb * 512:(tb + 1) * 512], in_=self.ps[b][0:H, :],
                                                              func=AF.Exp, bias=self.nbf[:], scale=-1.0),
                    reads=[("ps", b), "nbf"], writes=BK)
        self.wdone()
        self.op("act", lambda e: e.activation(out=spv, in_=ef, func=AF.Ln, bias=1.0, scale=1.0), reads=BK, writes=BK)
        self.op("dve", lambda e: e.tensor_tensor_scan(out=C, data0=onesf, data1=spv, initial=0.0, op0=ALU.mult, op1=ALU.add),
                reads=BK, writes=BK)
        self.op("dve", lambda e: e.tensor_scalar(out=caug, in0=C, scalar1=-1.0 / SCALE, scalar2=None, op0=ALU.mult),
                reads=BK, writes=self.XS_KEYS[0])
        self.op("sp", lambda e: e.dma_start(out=self.Cs, in_=caug), reads=self.XS_KEYS[0], writes=["Cs"], dsem="cs")
        b = self.galloc()
        for kt in range(NT):
            self.op("pe", lambda e, b=b, kt=kt: e.transpose(out=self.ps[b][:, kt * H:(kt + 1) * H],
                                                           in_=C[:, kt * 128:(kt + 1) * 128], identity=self.identf[0:H, 0:H]),
                    reads=BK + ["identf"], writes=[("ps", b)], signal=(kt == NT - 1))
        self.op("dve", lambda e, b=b: e.tensor_copy(out=self.cbias[:, :, :],
                                                    in_=self.ps[b][:, 0:NT * H].rearrange("p (k h) -> p k h", h=H)),
                reads=[("ps", b)], writes=["cbias"])

    def fox_load(self, pair):
        for i in range(2):
            hd = 2 * pair + i
            self.op("sp", lambda e, i=i, hd=hd: e.dma_start(out=self.KT[i][0:65, :], in_=self.Ks[hd]),
                    reads=[("Ks", hd), ("Ks1", hd)], writes=[("KT", i, tb) for tb in range(NB)], dsem=f"kt{i}")
            self.op("sp", lambda e, i=i, hd=hd: e.dma_start(out=self.QT[i][64:65, :], in_=self.Cs[hd:hd + 1, :]),
                    reads=["Cs"], writes=[("QTaug", i)], dsem=f"qa{i}")
        self.op("sp", lambda e, pair=pair: e.dma_start(out=self.Vp[:, :, :].rearrange("p t f -> p (t f)"), in_=self.Vs[pair]),
                reads=[("Vs", pair)], writes=[("Vp", g, i) for g in range(4) for i in range(2)], dsem="vp")

    def final(self):
        self.stats()
        for tt in range(NT):
            k = tt % 2
            self.op("dve", lambda e, tt=tt, k=k: e.scalar_tensor_tensor(
                out=self.xs[k], in0=self.h[:, tt, :], scalar=self.rstd[:, tt:tt + 1], in1=self.gfin[:],
                op0=ALU.mult, op1=ALU.mult), reads=[("h", tt), "rstd", "gfin"], writes=self.XS_KEYS[k])
            self.op("sp", lambda e, tt=tt, k=k: e.dma_start(out=self.out[tt * 128:(tt + 1) * 128, :], in_=self.xs[k]),
                    reads=self.XS_KEYS[k], writes=[("out", tt)], dsem=f"o{k}")
        self.finish(["o0", "o1"])

    def dump_h(self):
        for tt in range(NT):
            self.op("sp", lambda e, tt=tt: e.dma_start(out=self.out[tt * 128:(tt + 1) * 128, :], in_=self.h[:, tt, :]),
                    reads=[("h", tt)], writes=[("out", tt)], dsem="o0")
        self.finish(["o0"])

    def dump_dbg(self):
        nc = self.nc
        dv = nc.dram_tensor("dbg_v", [128, NT * 192], BF16, kind="ExternalOutput").ap()
        dq = nc.dram_tensor("dbg_q", [128, 2 * S], BF16, kind="ExternalOutput").ap()
        dk = nc.dram_tensor("dbg_k", [128, 2 * S], BF16, kind="ExternalOutput").ap()
        dr = nc.dram_tensor("dbg_r", [128, NSLOT * SLOT], BF16, kind="ExternalOutput").ap()
        self.op("sp", lambda e: e.dma_start(out=dv, in_=self.Vp[:, :, :].rearrange("p t f -> p (t f)")),
                reads=[("Vp", g, i) for g in range(4) for i in range(2)], dsem="o0")
        self.op("sp", lambda e: e.dma_start(out=dq, in_=self.QTall[:, :]),
                reads=self.XS_KEYS[0] + self.XS_KEYS[1], dsem="o0")
        self.op("sp", lambda e: e.dma_start(out=dk, in_=self.KTall[:, :]),
                reads=[("KT", i, tb) for i in range(2) for tb in range(NB)], dsem="o0")
        self.op("sp", lambda e: e.dma_start(out=dr, in_=self.ring[:, :]),
                reads=[("ring", s_, t) for s_ in range(NSLOT) for t in range(3)], dsem="o0")
        self.dump_h()

    def finish(self, keys):
        sp = self.engs["sp"]
        for k in list(keys) + [k for k in self.dcount if k not in keys]:
            if self.dcount[k] > 0:
                sp.h.wait_ge(self.sems[k], self.dcount[k])

    def build(self):
        with self.es:
            self.declare()
            self.setup()
            self.build_wlist()
            self._body()
        return self.nc

    def _body(self):
        for L in range(DEPTH):
            if L == N_A:
                self.shared_kv()
                if self.stop == "kv":
                    return self.dump_h()
            if self.stop == "setup":
                return self.dump_h()
            self.norm(L)
            if self.stop == "norm":
                return self.dump_h()
            self.gset = self.G_ATTN
            for pair in (self.pair_order if L < N_A else range(8)):
                if L < N_A:
                    s, wv = self.wget("qkv")
                    self.wreads = [("ring", s, 0)]
                    self.proj_T(wv[0], 0, self.QT, "QT")
                    self.wreads = [("ring", s, 1)]
                    self.proj_T(wv[1], 0, self.KT, "KT")
                    self.wreads = [("ring", s, 2)]
                    self.proj_V(wv[2], 0)
                    self.wdone()
                    if self.stop == "proj":
                        return self.dump_h()
                    self.moba_gate(pair)
                    grp = [(i, j) for j in (0, 1) for i in range(2)] + [(i, j) for j in (2, 3) for i in range(2)]
                    self.attention(pair, 72, self.abias, "abias", groups=grp,
                                   hook=lambda pair=pair: self.moba_gate2(pair))
                    if self.stop == "att":
                        return self.dump_h()
                else:
                    s, wv = self.wget("q")
                    self.wreads = [("ring", s, 0)]
                    self.fox_load(pair)
                    self.proj_T(wv[0], 0, self.QT, "QT")
                    self.wdone()
                    self.attention(pair, 65, self.cbias, "cbias")
            self.gset = self.G_FULL
            self.wo()
            if self.stop == f"attn{L}":
                return self.dump_h()
            self.norm(4 + L)
            self.mlp()
            if self.stop == f"mlp{L}":
                return self.dump_h()
        self.final()


def _consts():
    bf = ml_dtypes.bfloat16
    slopes = (2.0 ** (-8.0 * np.arange(1, H + 1) / H)).astype(np.float32)
    p = np.arange(128)
    c = {}
    c["identf"] = np.eye(128, dtype=np.float32)
    c["identb"] = np.eye(128, dtype=np.float32).astype(bf)
    c["trimask"] = np.where(p[:, None] > p[None, :], NEG, 0.0).astype(np.float32).astype(bf)
    c["kblock"] = (np.arange(S)[None, :] // 256 == np.arange(8)[:, None]).astype(np.float32).astype(bf)
    pos = (128 * np.arange(NT)[None, :] + p[:, None]).astype(np.float32)
    c["abias"] = np.ascontiguousarray((pos[:, :, None] * slopes[None, None, :]).astype(np.float32))
    aq = (-(1.0 / SCALE) * slopes[:, None] * np.arange(S, dtype=np.float32)[None, :]).astype(np.float32)
    c["augq"] = np.ascontiguousarray(np.broadcast_to(aq[:, None, :], (H, 8, S))).astype(bf)
    gm = np.zeros((128, 8, 2, 8), np.float32)
    gv = np.zeros((128, 8, 2, 8), np.float32)
    for qq in range(8):
        b = (8 + qq) // 2
        gm[:, qq, :, b:] = -1e30
        gv[:, qq, :, :b] = 1.0
    c["gmask"] = gm.reshape(128, 128)
    c["gvalid"] = gv.reshape(128, 128)
    return c


_CACHE = {}


def _get_nc(stop=None):
    if stop not in _CACHE:
        _CACHE[stop] = B(stop).build()
    return _CACHE[stop]


def _in_maps(inputs, cores):
    f = lambda a: np.ascontiguousarray(np.asarray(a, dtype=np.float32))
    gt = np.concatenate([f(inputs["attn_norm"]), f(inputs["mlp_norm"]), f(inputs["kv_norm"])[None, :]], axis=0)
    gtab = np.ascontiguousarray(gt.reshape(9, KC, 128).transpose(2, 0, 1))
    def pairwise(w, nt):
        lead = w.shape[:-2]
        w = w.reshape(lead + (KC, 128, nt, 8, 128))
        nl = len(lead)
        perm = tuple(range(nl)) + (nl + 3, nl + 1, nl + 2, nl + 0, nl + 4)
        return np.ascontiguousarray(w.transpose(perm)).reshape(lead + (8, 128, nt * KC * 128))
    shared = {
        "wqkv_r": pairwise(f(inputs["moba_w_qkv"]), 3), "wq_r": pairwise(f(inputs["fox_w_q"]), 1), "w_o": f(inputs["w_o"]),
        "wkv_r": pairwise(f(inputs["w_kv"]), 2),
        "wf_r": np.ascontiguousarray(f(inputs["w_f"]).reshape(KC, 128, H).transpose(1, 0, 2)).reshape(128, KC * H),
        "b_f": f(inputs["b_f"]).reshape(H, 1),
        "w_up": f(inputs["w_up"]), "w_down": f(inputs["w_down"]), "final_norm": f(inputs["final_norm"]),
        "gtab": gtab,
    }
    shared.update(_consts())
    x = f(inputs["x"])
    return [dict(shared, x=np.ascontiguousarray(x[b])) for b in cores]


def kernel(**inputs):
    nc = _get_nc(None)
    maps = _in_maps(inputs, list(range(8)))
    res = run_bass_kernel_spmd(nc, maps, core_ids=list(range(8)))
    return np.stack([np.asarray(r["out"], dtype=np.float32) for r in res.results], axis=0)
```

```python
import contextlib
import numpy as np
import ml_dtypes
import concourse.bass as bass
import concourse.mybir as mybir
from concourse.bass_utils import run_bass_kernel_spmd

F32 = mybir.dt.float32
BF16 = mybir.dt.bfloat16
AF = mybir.ActivationFunctionType
ALU = mybir.AluOpType
AX = mybir.AxisListType

S = 2048
D = 1024
H = 16
DH = 64
FF = 4096
NT = 16
NB = 4
KC = 8
NSLOT = 4
SLOT = 4096
NEG = -30000.0
EPS = 1e-6
SCALE = 0.125
N_A = 2
DEPTH = 4


class Eng:
    def __init__(self, name, h):
        self.name = name
        self.h = h
        self.nsig = 0
        self.seen = {}


class B:
    def __init__(self, stop=None):
        self.stop = stop
        import os as _os
        self.pair_order = list(range(8))[::-1] if _os.environ.get("KREV") else list(range(8))
        self.nc = bass.Bass("TRN2", target_bir_lowering=False)
        self.es = contextlib.ExitStack()
        nc = self.nc
        self.engs = {"pe": Eng("pe", nc.tensor), "act": Eng("act", nc.scalar), "dve": Eng("dve", nc.vector),
                     "pool": Eng("pool", nc.gpsimd), "sp": Eng("sp", nc.sync)}
        self.sems = {}
        self.dcount = {}
        for n in self.engs:
            self.sems[n] = self.es.enter_context(nc.semaphore("s_" + n))
        self.res = {}
        self.const_keys = set()
        self.gbank = 0
        self.G_FULL = [0, 1, 2, 3, 4, 5, 6]
        self.G_ATTN = [5, 6]
        self.gset = self.G_FULL

    def dsem(self, name):
        if name not in self.sems:
            self.sems[name] = self.es.enter_context(self.nc.semaphore("d_" + name))
            self.dcount[name] = 0
        return name

    def sb(self, name, shape, dt):
        return self.es.enter_context(self.nc.sbuf_tensor(name, shape, dt))

    def _wait(self, eng, ev):
        key, val = ev
        if eng.seen.get(key, 0) >= val:
            return
        eng.h.wait_ge(self.sems[key], val)
        eng.seen[key] = val

    def op(self, en, fn, reads=(), writes=(), signal=True, dsem=None):
        eng = self.engs[en]
        evs = []
        for r in reads:
            st = self.res.get(r)
            if st is not None and st[0] is not None:
                evs.append(st[0])
        for w in writes:
            st = self.res.get(w)
            if st is not None:
                if st[0] is not None:
                    evs.append(st[0])
                evs.extend(st[1].items())
        for ev in evs:
            if ev[0] == "pe" and en == "pe":
                continue
            if ev[0] in self.engs:
                assert self.engs[ev[0]].nsig >= ev[1], ("unsignaled dependency", en, ev, reads, writes)
            self._wait(eng, ev)
        ins = fn(eng.h)
        if dsem is not None:
            self.dsem(dsem)
            ins.then_inc(self.sems[dsem], 16)
            self.dcount[dsem] += 16
            ev = (dsem, self.dcount[dsem])
        elif signal:
            ins.then_inc(self.sems[en], 1)
            eng.nsig += 1
            ev = (en, eng.nsig)
        else:
            ev = (en, eng.nsig + 1)
        for w in writes:
            self.res[w] = [ev, {}]
        for r in reads:
            if r in self.const_keys:
                continue
            st = self.res.setdefault(r, [None, {}])
            if st[1].get(ev[0], 0) < ev[1]:
                st[1][ev[0]] = ev[1]
        return ev

    def galloc(self):
        b = self.gset[self.gbank % len(self.gset)]
        self.gbank += 1
        return b

    def declare(self):
        nc = self.nc
        di = lambda n, s, d=F32: nc.dram_tensor(n, s, d, kind="ExternalInput").ap()
        self.x = di("x", [S, D])
        self.w_qkv = di("wqkv_r", [N_A, 8, 128, 3 * KC * 128])
        self.w_q = di("wq_r", [DEPTH - N_A, 8, 128, KC * 128])
        self.w_o = di("w_o", [DEPTH, D, D])
        self.w_kv = di("wkv_r", [8, 128, 2 * KC * 128])
        self.w_f = di("wf_r", [128, KC * H])
        self.b_f = di("b_f", [H, 1])
        self.w_up = di("w_up", [DEPTH, D, FF])
        self.w_down = di("w_down", [DEPTH, FF, D])
        self.final_norm = di("final_norm", [D])
        self.gtab_d = di("gtab", [128, 9, KC])
        self.identf_d = di("identf", [128, 128])
        self.identb_d = di("identb", [128, 128], BF16)
        self.trimask_d = di("trimask", [128, 128], BF16)
        self.kblock_d = di("kblock", [8, S], BF16)
        self.abias_d = di("abias", [128, NT, H])
        self.augq_d = di("augq", [H, 8, S], BF16)
        self.gmask_d = di("gmask", [128, 128])
        self.gvalid_d = di("gvalid", [128, 128])
        self.Ks = nc.dram_tensor("Ks", [H, 65, S], BF16, kind="Internal").ap()
        self.Vs = nc.dram_tensor("Vs", [H // 2, 128, NT * 192], BF16, kind="Internal").ap()
        self.Cs = nc.dram_tensor("Cs", [H, S], BF16, kind="Internal").ap()
        self.out = nc.dram_tensor("out", [S, D], F32, kind="ExternalOutput").ap()

        sb = self.sb
        self.h = sb("h", [128, NT, D], F32)
        self.hnT = sb("hnT", [128, KC, S], BF16)
        self.big = sb("big", [128, 16384], BF16)
        self.ring = sb("ring", [128, NSLOT * SLOT], BF16)
        self.QTall = sb("QTall", [128, 2 * S], BF16)
        self.KTall = sb("KTall", [128, 2 * S], BF16)
        self.Vp = sb("Vp", [128, NT, 192], BF16)
        self.PT = sb("PT", [128, 4, 512], BF16)
        self.rc = sb("rc", [128, 2, 512], F32)
        self.gfin = sb("gfin", [128, D], F32)
        self.gT = sb("gT", [128, 9, KC], F32)
        self.identf = sb("identf_s", [128, 128], F32)
        self.identb = sb("identb_s", [128, 128], BF16)
        self.trimask = sb("trimask_s", [128, 128], BF16)
        self.abias = sb("abias_s", [128, NT, H], F32)
        self.cbias = sb("cbias_s", [128, NT, H], F32)
        self.gmask = sb("gmask_s", [128, 128], F32)
        self.gvalid = sb("gvalid_s", [128, 128], F32)
        self.ss = sb("ss", [128, NT], F32)
        self.rt = sb("rt", [128, NT], F32)
        self.rstd = sb("rstd", [128, NT], F32)
        self.bft = sb("bft", [H, 1], F32)
        self.nbf = sb("nbf", [H, 1], F32)
        self.kmf = sb("kmf", [64, 2, 8], F32)
        self.kmT = sb("kmT", [64, 2, 8], BF16)
        self.g2 = sb("g2", [128, 128], F32)
        self.top8 = sb("top8", [128, 128], F32)
        self.selt = sb("selt", [128, 128], F32)
        self.augtok = sb("augtok", [128, 64 + 128], F32)
        self.onesr = sb("onesr", [1, S], BF16)
        self.ps = [self.es.enter_context(nc.psum_tensor(f"ps{b}", [128, 512], F32)) for b in range(8)]

        self.QT = [self.QTall[:, i * S:(i + 1) * S] for i in range(2)]
        self.KT = [self.KTall[:, i * S:(i + 1) * S] for i in range(2)]
        qf = self.QTall[:, :].bitcast(F32)
        self.xs = [qf[:, k * 1024:(k + 1) * 1024] for k in range(2)]
        self.mixT = self.big[:, :].rearrange("p (c t) -> p c t", t=S)
        self.uT = [self.big[:, b * 8192:(b + 1) * 8192].rearrange("p (f t) -> p f t", t=S) for b in range(2)]
        self.bigf = self.big[:, :].bitcast(F32)
        self.XS_KEYS = [[("QT", k, tb) for tb in range(NB)] + [("QTaug", k)] for k in range(2)]
        self.BIG_KEYS = ([("uT", b, fs, tb) for b in range(2) for fs in range(4) for tb in range(NB)]
                         + [("mixT", p, i, j) for p in range(8) for i in range(2) for j in range(NB)])

    def setup(self):
        c = self.dsem("c")
        consts = [
            (self.identf[:], self.identf_d, "identf"), (self.identb[:], self.identb_d, "identb"),
            (self.trimask[:], self.trimask_d, "trimask"), (self.abias[:], self.abias_d, "abias"),
            (self.gmask[:], self.gmask_d, "gmask"), (self.gvalid[:], self.gvalid_d, "gvalid"),
            (self.gT[:], self.gtab_d, "gT"), (self.gfin[:], self.final_norm.partition_broadcast(128), "gfin"),
            (self.bft[:], self.b_f, "bft"),
            (self.KT[0][64:72, :], self.kblock_d, "KTaug0"), (self.KT[1][64:72, :], self.kblock_d, "KTaug1"),
        ]
        for (o, i, k) in consts:
            self.op("sp", lambda e, o=o, i=i: e.dma_start(out=o, in_=i), writes=[k], dsem=c)
        tot = self.dcount[c]
        for (_, _, k) in consts:
            self.res[k] = [(c, tot), {}]
            if k != "bft":
                self.const_keys.add(k)
        xv = self.x.rearrange("(t p) f -> p t f", p=128)
        for g in range(4):
            self.op("sp", lambda e, g=g: e.dma_start(out=self.h[:, 4 * g:4 * g + 4, :], in_=xv[:, 4 * g:4 * g + 4, :]),
                    writes=[("h", tt) for tt in range(4 * g, 4 * g + 4)], dsem=f"x{g}")
        self.op("dve", lambda e: e.memset(self.Vp[:, :, 64:128], 1.0), writes=["Vones"])
        self.op("dve", lambda e: e.memset(self.augtok[:, 0:64], 0.0), writes=["augpad"])
        self.op("dve", lambda e: e.memset(self.onesr[:], 1.0), writes=["onesr"])
        self.op("dve", lambda e: e.tensor_scalar(out=self.nbf[:], in0=self.bft[:], scalar1=-1.0, scalar2=None,
                                                 op0=ALU.mult), reads=["bft"], writes=["nbf"])
        for k in ("Vones", "augpad", "onesr", "nbf"):
            self.const_keys.add(k)

    def ring_view(self, s, kc, f):
        return self.ring[:, s * SLOT:s * SLOT + kc * f].rearrange("p (k f) -> p k f", f=f)

    def build_wlist(self):
        wl = []
        rr = lambda ap: ap.rearrange("(k p) f -> p k f", p=128)

        def mlp(L):
            ups = [("up", [(0, (KC, 512), None, rr(self.w_up[L])[:, :, c * 512:(c + 1) * 512])]) for c in range(8)]
            dns = [("down", [(0, (4, D), None, rr(self.w_down[L][c * 512:(c + 1) * 512, :]))]) for c in range(8)]
            order = [ups[0]]
            for c in range(8):
                if c + 1 < 8:
                    order.append(ups[c + 1])
                order.append(dns[c])
            return order

        def wo(L):
            return [("wo", [(0, (KC, 512), None, rr(self.w_o[L])[:, :, hf * 512:(hf + 1) * 512])]) for hf in range(2)]

        for L in range(N_A):
            for p in self.pair_order:
                wl.append(("qkv", [((0, 1, 2), 3 * KC * 128, 3, self.w_qkv[L, p])]))
            wl += wo(L) + mlp(L)
        for p in range(8):
            wl.append(("kv", [((0, 1), 2 * KC * 128, 2, self.w_kv[p])]))
        wl.append(("wf", [((0,), KC * H, 0, self.w_f)]))
        for L in range(N_A, DEPTH):
            for p in range(8):
                wl.append(("q", [((0,), KC * 128, 1, self.w_q[L - N_A, p])]))
            wl += wo(L) + mlp(L)
        self.wl = wl
        self.wpos = 0
        self.wfill = 0
        for _ in range(NSLOT):
            self.fill_next()

    def fill_next(self):
        if self.wfill >= len(self.wl):
            return
        i = self.wfill
        self.wfill += 1
        s = i % NSLOT
        name, parts = self.wl[i]
        for (part, sz, nt, src) in parts:
            if nt is None:
                kc, f = sz
                dst = self.ring_view(s, kc, f)
            else:
                dst = self.ring[:, s * SLOT:s * SLOT + sz]
            keys = [("ring", s, t) for t in range(3)]
            self.op("pool", lambda e, dst=dst, src=src: e.dma_start(out=dst, in_=src), writes=keys, dsem=f"r{s}")

    def part_view(self, s, t):
        return self.ring[:, s * SLOT + t * 1024:s * SLOT + (t + 1) * 1024].rearrange("p (k c) -> p k c", c=128)

    def wget(self, name):
        i = self.wpos
        self.wpos += 1
        assert self.wl[i][0] == name, (self.wl[i][0], name)
        s = i % NSLOT
        parts = self.wl[i][1]
        nt = parts[0][2]
        if nt is not None:
            if nt == 0:
                return s, self.ring[:, s * SLOT:s * SLOT + KC * H].rearrange("p (k h) -> p k h", h=H)
            return s, [self.part_view(s, t) for t in range(nt)]
        kc, f = parts[0][1]
        return s, self.ring_view(s, kc, f)

    def wdone(self):
        self.fill_next()

    def stats(self):
        self.op("dve", lambda e: e.memset(self.ss[:], 0.0), writes=["ss"])
        for tt in range(NT):
            k = tt % 2
            self.op("act", lambda e, tt=tt, k=k: e.activation(out=self.xs[k], in_=self.h[:, tt, :], func=AF.Square,
                                                              accum_out=self.ss[:, tt:tt + 1]),
                    reads=[("h", tt), "ss"], writes=self.XS_KEYS[k] + [("ssc", tt)])
        self.op("act", lambda e: e.activation(out=self.rt[:], in_=self.ss[:], func=AF.Sqrt, bias=EPS, scale=1.0 / D),
                reads=[("ssc", tt) for tt in range(NT)] + ["ss"], writes=["rt"])
        self.op("dve", lambda e: e.reciprocal(out=self.rstd[:], in_=self.rt[:]), reads=["rt"], writes=["rstd"])

    def norm(self, gi):
        self.stats()
        for tt in range(NT):
            k = tt % 2
            self.op("act", lambda e, tt=tt, k=k: e.activation(out=self.xs[k], in_=self.h[:, tt, :], func=AF.Copy,
                                                              scale=self.rstd[:, tt:tt + 1]),
                    reads=[("h", tt), "rstd"], writes=self.XS_KEYS[k])
            for g in range(2):
                b = self.galloc()
                for q in range(4):
                    kc = 4 * g + q
                    self.op("pe", lambda e, b=b, q=q, kc=kc, k=k: e.transpose(
                        out=self.ps[b][:, q * 128:(q + 1) * 128], in_=self.xs[k][:, kc * 128:(kc + 1) * 128],
                        identity=self.identf[:]),
                        reads=self.XS_KEYS[k] + ["identf"], writes=[("ps", b)], signal=(q == 3))
                self.op("dve", lambda e, b=b, g=g, tt=tt: e.tensor_tensor(
                    out=self.hnT[:, 4 * g:4 * g + 4, tt * 128:(tt + 1) * 128],
                    in0=self.ps[b][:, :].rearrange("p (k t) -> p k t", t=128),
                    in1=self.gT[:, gi, 4 * g:4 * g + 4].unsqueeze(2).to_broadcast([128, 4, 128]), op=ALU.mult),
                    reads=[("ps", b), "gT"], writes=[("hnT", tt)])

    def proj_T(self, wv, col0, dst, dkey):
        for tb in range(NB):
            b = self.galloc()
            for kc in range(KC):
                self.op("pe", lambda e, b=b, kc=kc, tb=tb: e.matmul(
                    self.ps[b][:, :], lhsT=wv[:, kc, col0:col0 + 128], rhs=self.hnT[:, kc, tb * 512:(tb + 1) * 512],
                    start=(kc == 0), stop=(kc == KC - 1)),
                    reads=[("hnT", tt) for tt in range(4 * tb, 4 * tb + 4)] + self.wreads, writes=[("ps", b)],
                    signal=(kc == KC - 1))
            self.op("act", lambda e, b=b, tb=tb: e.copy(out=dst[0][0:64, tb * 512:(tb + 1) * 512], in_=self.ps[b][0:64, :]),
                    reads=[("ps", b)], writes=[(dkey, 0, tb), ("psx", b)])
            self.op("dve", lambda e, b=b, tb=tb: e.tensor_copy(out=dst[1][0:64, tb * 512:(tb + 1) * 512],
                                                               in_=self.ps[b][64:128, :]),
                    reads=[("ps", b)], writes=[(dkey, 1, tb), ("psx", b)])

    def proj_V(self, wv, col0):
        for g in range(4):
            b = self.galloc()
            for tl in range(4):
                tt = 4 * g + tl
                for kc in range(KC):
                    self.op("pe", lambda e, b=b, tl=tl, tt=tt, kc=kc: e.matmul(
                        self.ps[b][:, tl * 128:(tl + 1) * 128], lhsT=self.hnT[:, kc, tt * 128:(tt + 1) * 128],
                        rhs=wv[:, kc, col0:col0 + 128], start=(kc == 0), stop=(kc == KC - 1), skip_group_check=True),
                        reads=[("hnT", tt)] + self.wreads, writes=[("ps", b)], signal=(kc == KC - 1 and tl == 3))
            pv = self.ps[b][:, :].rearrange("p (t f) -> p t f", f=128)
            self.op("act", lambda e, pv=pv, g=g: e.copy(out=self.Vp[:, 4 * g:4 * g + 4, 0:64], in_=pv[:, :, 0:64]),
                    reads=[("ps", b)], writes=[("Vp", g, 0), ("psx", b)])
            self.op("dve", lambda e, pv=pv, g=g: e.tensor_copy(out=self.Vp[:, 4 * g:4 * g + 4, 128:192], in_=pv[:, :, 64:128]),
                    reads=[("ps", b)], writes=[("Vp", g, 1), ("psx", b)])

    def moba_gate(self, pair):
        for i in range(2):
            hd = 2 * pair + i
            self.op("sp", lambda e, i=i, hd=hd: e.dma_start(out=self.QT[i][64:72, :], in_=self.augq_d[hd]),
                    writes=[("QTaug", i)], dsem=f"qa{i}")
            self.op("dve", lambda e, i=i: e.tensor_reduce(out=self.kmf[:, i, :],
                                                          in_=self.KT[i][0:64, :].rearrange("p (n k) -> p n k", k=256),
                                                          axis=AX.X, op=ALU.add),
                    reads=[("KT", i, tb) for tb in range(NB)], writes=[("kmf", i)])
            self.op("dve", lambda e, i=i: e.tensor_scalar(out=self.kmT[:, i, :], in0=self.kmf[:, i, :], scalar1=1.0 / 256,
                                                          scalar2=None, op0=ALU.mult),
                    reads=[("kmf", i)], writes=[("kmT", i)])
        gb = self.galloc()
        for qq in range(8):
            for i in range(2):
                grp = qq * 2 + i
                qt = 8 + qq
                self.op("pe", lambda e, i=i, grp=grp, qt=qt: e.matmul(
                    self.ps[gb][:, grp * 8:(grp + 1) * 8], lhsT=self.QT[i][0:64, qt * 128:(qt + 1) * 128],
                    rhs=self.kmT[:, i, :], start=True, stop=True, skip_group_check=True),
                    reads=[("QT", i, qt // 4), ("kmT", i)], writes=[("ps", gb)], signal=(grp == 15))
        self.op("dve", lambda e: e.tensor_tensor(out=self.g2[:], in0=self.ps[gb][:, 0:128], in1=self.gmask[:], op=ALU.add),
                reads=[("ps", gb), "gmask"], writes=["g2"])
        for grp in range(16):
            self.op("dve", lambda e, grp=grp: e.max(out=self.top8[:, grp * 8:(grp + 1) * 8], in_=self.g2[:, grp * 8:(grp + 1) * 8]),
                    reads=["g2"], writes=["top8"])
        g3 = lambda t: t[:, :].rearrange("p (g n) -> p g n", n=8)
        self.op("dve", lambda e: e.tensor_tensor(out=g3(self.selt), in0=g3(self.g2),
                                                 in1=g3(self.top8)[:, :, 2:3].to_broadcast([128, 16, 8]), op=ALU.is_ge),
                reads=["g2", "top8"], writes=["selt"])
        self.op("dve", lambda e: e.tensor_scalar(out=self.selt[:], in0=self.selt[:], scalar1=1.0, scalar2=-NEG,
                                                 op0=ALU.subtract, op1=ALU.mult), reads=["selt"], writes=["selt"])
        self.op("dve", lambda e: e.tensor_tensor(out=self.augtok[:, 64:192], in0=self.selt[:], in1=self.gvalid[:], op=ALU.mult),
                reads=["selt", "gvalid"], writes=["augtok"])

    def moba_gate2(self, pair):
        for i in range(2):
            for jj in range(2):
                b = self.galloc()
                for q in range(4):
                    grp = (4 * jj + q) * 2 + i
                    self.op("pe", lambda e, b=b, q=q, grp=grp: e.transpose(
                        out=self.ps[b][0:72, q * 128:(q + 1) * 128], in_=self.augtok[:, 8 * grp:8 * grp + 72],
                        identity=self.identf[:]),
                        reads=["augtok", "identf", "augpad"], writes=[("ps", b)], signal=(q == 3))
                c0 = 1024 + 512 * jj
                self.op("dve", lambda e, b=b, i=i, c0=c0: e.tensor_tensor(
                    out=self.QT[i][64:72, c0:c0 + 512], in0=self.ps[b][64:72, :], in1=self.QT[i][64:72, c0:c0 + 512],
                    op=ALU.add), reads=[("ps", b), ("QTaug", i)], writes=[("QTaug", i)])

    def attention(self, pair, K, btab, bkey, groups=None, hook=None):
        if groups is None:
            groups = [(i, j) for i in range(2) for j in range(NB)]
        blocks = [(i, j, kt) for (i, j) in groups for kt in range(4 * j + 4)]
        nblk = len(blocks)
        hook_idx = None
        if hook is not None:
            hook_idx = min(n for n, (i, j, kt) in enumerate(blocks) if j >= 2)

        SB = [0, 1, 4]

        def qk(n):
            if n == hook_idx:
                hook()
            i, j, kt = blocks[n]
            sbk = SB[n % 3]
            r = kt - 4 * j
            off = 128 * r if r > 0 else 0
            N = 512 - off
            diag = r >= 0
            rd = [("QT", i, j), ("QTaug", i), ("KT", i, kt // 4), f"KTaug{i}"]
            self.op("pe", lambda e: e.matmul(self.ps[sbk][:, 0:N], lhsT=self.KT[i][0:K, kt * 128:(kt + 1) * 128],
                                            rhs=self.QT[i][0:K, 512 * j + off:512 * j + 512], start=True, stop=(not diag)),
                    reads=rd, writes=[("ps", sbk)], signal=(not diag))
            if diag:
                self.op("pe", lambda e: e.matmul(self.ps[sbk][:, 0:128], lhsT=self.identb[:], rhs=self.trimask[:],
                                                start=False, stop=True),
                        reads=["identb", "trimask"], writes=[("ps", sbk)], signal=True)

        def exp_pv(n, gidx):
            i, j, kt = blocks[n]
            sbk = SB[n % 3]
            pb = n % 4
            ab = 2 + gidx % 2
            r = kt - 4 * j
            off = 128 * r if r > 0 else 0
            N = 512 - off
            hd = 2 * pair + i
            self.op("act", lambda e: e.activation(out=self.PT[:, pb, 0:N], in_=self.ps[sbk][:, 0:N], func=AF.Exp,
                                                  bias=btab[:, kt, hd:hd + 1], scale=SCALE),
                    reads=[("ps", sbk), bkey], writes=[("PT", pb)])
            last = (kt == 4 * j + 3)
            self.op("pe", lambda e: e.matmul(self.ps[ab][:, off:512], lhsT=self.Vp[:, kt, 64 * i:64 * i + 128],
                                            rhs=self.PT[:, pb, 0:N], start=(kt == 0), stop=last),
                    reads=[("PT", pb), ("Vp", kt // 4, i), "Vones"], writes=[("ps", ab)], signal=last)
            self.op("pe", lambda e: e.matmul(self.ps[7][:, 0:256], lhsT=self.identb[:], rhs=self.hnT[:, 0, 0:256],
                                            start=True, stop=True), reads=["identb"], writes=[("ps", 7)], signal=False)
            if last:
                x = gidx % 2
                num = slice(0, 64) if i == 0 else slice(64, 128)
                den = slice(64, 128) if i == 0 else slice(0, 64)
                self.op("dve", lambda e: e.reciprocal(out=self.rc[num, x, :], in_=self.ps[ab][den, :]),
                        reads=[("ps", ab)], writes=[("rc", x)])
                self.op("dve", lambda e: e.tensor_tensor(out=self.mixT[num, pair, 512 * j:512 * j + 512],
                                                         in0=self.ps[ab][num, :], in1=self.rc[num, x, :], op=ALU.mult),
                        reads=[("ps", ab), ("rc", x)], writes=[("mixT", pair, i, j)])

        gidx = 0
        qk(0)
        qk(1)
        for n in range(nblk):
            if n + 2 < nblk:
                qk(n + 2)
            exp_pv(n, gidx)
            i, j, kt = blocks[n]
            if kt == 4 * j + 3:
                gidx += 1

    def wo(self):
        for hf in range(2):
            s, wv = self.wget("wo")
            self.wreads = [("ring", s, 0)]
            for tt in range(NT):
                b = self.galloc()
                for kc in range(KC):
                    self.op("pe", lambda e, b=b, kc=kc, tt=tt: e.matmul(
                        self.ps[b][:, :], lhsT=self.mixT[:, kc, tt * 128:(tt + 1) * 128], rhs=wv[:, kc, :],
                        start=(kc == 0), stop=(kc == KC - 1)),
                        reads=[("mixT", kc, i, tt // 4) for i in range(2)] + self.wreads, writes=[("ps", b)],
                        signal=(kc == KC - 1))
                self.op("dve", lambda e, b=b, tt=tt, hf=hf: e.tensor_tensor(
                    out=self.h[:, tt, hf * 512:(hf + 1) * 512], in0=self.ps[b][:, :],
                    in1=self.h[:, tt, hf * 512:(hf + 1) * 512], op=ALU.add),
                    reads=[("ps", b), ("h", tt)], writes=[("h", tt)])
            self.wdone()

    def mlp_up(self, buf):
        s, wv = self.wget("up")
        wr = [("ring", s, 0)]
        for fs in range(4):
            for tb in range(NB):
                b = self.galloc()
                for kc in range(KC):
                    self.op("pe", lambda e, b=b, kc=kc, fs=fs, tb=tb: e.matmul(
                        self.ps[b][:, :], lhsT=wv[:, kc, fs * 128:(fs + 1) * 128], rhs=self.hnT[:, kc, tb * 512:(tb + 1) * 512],
                        start=(kc == 0), stop=(kc == KC - 1)),
                        reads=[("hnT", tt) for tt in range(4 * tb, 4 * tb + 4)] + wr, writes=[("ps", b)],
                        signal=(kc == KC - 1))
                x = (fs * NB + tb) % 2
                self.op("act", lambda e, b=b, x=x: e.activation(out=self.rc[:, x, :], in_=self.ps[b][:, :], func=AF.Relu),
                        reads=[("ps", b)], writes=[("rc", x)])
                self.op("dve", lambda e, x=x, fs=fs, tb=tb: e.tensor_tensor(
                    out=self.uT[buf][:, fs, tb * 512:(tb + 1) * 512], in0=self.rc[:, x, :], in1=self.rc[:, x, :], op=ALU.mult),
                    reads=[("rc", x)], writes=[("uT", buf, fs, tb)])
        self.wdone()

    def mlp_down(self, buf):
        s, wv = self.wget("down")
        wr = [("ring", s, 0)]
        for tt in range(NT):
            for hf in range(2):
                b = self.galloc()
                for fs in range(4):
                    self.op("pe", lambda e, b=b, fs=fs, tt=tt, hf=hf: e.matmul(
                        self.ps[b][:, :], lhsT=self.uT[buf][:, fs, tt * 128:(tt + 1) * 128],
                        rhs=wv[:, fs, hf * 512:(hf + 1) * 512], start=(fs == 0), stop=(fs == 3)),
                        reads=[("uT", buf, fs, tt // 4)] + wr, writes=[("ps", b)], signal=(fs == 3))
                self.op("dve", lambda e, b=b, tt=tt, hf=hf: e.tensor_tensor(
                    out=self.h[:, tt, hf * 512:(hf + 1) * 512], in0=self.ps[b][:, :],
                    in1=self.h[:, tt, hf * 512:(hf + 1) * 512], op=ALU.add),
                    reads=[("ps", b), ("h", tt)], writes=[("h", tt)])
        self.wdone()

    def mlp(self):
        self.mlp_up(0)
        for c in range(8):
            if c + 1 < 8:
                self.mlp_up((c + 1) % 2)
            self.mlp_down(c % 2)

    def shared_kv(self):
        self.norm(8)
        for pair in range(8):
            s, wv = self.wget("kv")
            self.wreads = [("ring", s, 0)]
            self.proj_T(wv[0], 0, self.KT, "KT")
            self.wreads = [("ring", s, 1)]
            self.proj_V(wv[1], 0)
            self.wdone()
            for i in range(2):
                hd = 2 * pair + i
                self.op("sp", lambda e, i=i, hd=hd: e.dma_start(out=self.Ks[hd][0:64, :], in_=self.KT[i][0:64, :]),
                        reads=[("KT", i, tb) for tb in range(NB)], writes=[("Ks", hd)], dsem=f"ks{i}")
                self.op("sp", lambda e, hd=hd: e.dma_start(out=self.Ks[hd][64:65, :], in_=self.onesr[:]),
                        reads=["onesr"], writes=[("Ks1", hd)], dsem="kone")
            self.op("sp", lambda e, pair=pair: e.dma_start(out=self.Vs[pair], in_=self.Vp[:, :, :].rearrange("p t f -> p (t f)")),
                    reads=[("Vp", g, i) for g in range(4) for i in range(2)] + ["Vones"], writes=[("Vs", pair)], dsem="vs")
        for hd in range(H):
            self.res[("Ks1", hd)][0] = ("kone", self.dcount["kone"])
        s, wv = self.wget("wf")
        ef = self.bigf[0:H, 0:S]
        spv = self.bigf[0:H, S:2 * S]
        C = self.bigf[0:H, 2 * S:3 * S]
        onesf = self.bigf[0:H, 3 * S:4 * S]
        caug = self.QTall[0:H, 0:S]
        BK = self.BIG_KEYS
        self.op("dve", lambda e: e.memset(onesf, 1.0), writes=BK)
        for tb in range(NB):
            b = self.galloc()
            for kc in range(KC):
                self.op("pe", lambda e, b=b, kc=kc, tb=tb: e.matmul(
                    self.ps[b][0:H, :], lhsT=wv[:, kc, :], rhs=self.hnT[:, kc, tb * 512:(tb + 1) * 512],
                    start=(kc == 0), stop=(kc == KC - 1)),
                    reads=[("hnT", tt) for tt in range(4 * tb, 4 * tb + 4)] + [("ring", s, 0)], writes=[("ps", b)],
                    signal=(kc == KC - 1))
            self.op("act", lambda e, b=b, tb=tb: e.activation(out=ef[:, tb * 512:(tb + 1) * 512], in_=self.ps[b][0:H, :],
                                                              func=AF.Exp, bias=self.nbf[:], scale=-1.0),
                    reads=[("ps", b), "nbf"], writes=BK)
        self.wdone()
        self.op("act", lambda e: e.activation(out=spv, in_=ef, func=AF.Ln, bias=1.0, scale=1.0), reads=BK, writes=BK)
        self.op("dve", lambda e: e.tensor_tensor_scan(out=C, data0=onesf, data1=spv, initial=0.0, op0=ALU.mult, op1=ALU.add),
                reads=BK, writes=BK)
        self.op("dve", lambda e: e.tensor_scalar(out=caug, in0=C, scalar1=-1.0 / SCALE, scalar2=None, op0=ALU.mult),
                reads=BK, writes=self.XS_KEYS[0])
        self.op("sp", lambda e: e.dma_start(out=self.Cs, in_=caug), reads=self.XS_KEYS[0], writes=["Cs"], dsem="cs")
        b = self.galloc()
        for kt in range(NT):
            self.op("pe", lambda e, b=b, kt=kt: e.transpose(out=self.ps[b][:, kt * H:(kt + 1) * H],
                                                           in_=C[:, kt * 128:(kt + 1) * 128], identity=self.identf[0:H, 0:H]),
                    reads=BK + ["identf"], writes=[("ps", b)], signal=(kt == NT - 1))
        self.op("dve", lambda e, b=b: e.tensor_copy(out=self.cbias[:, :, :],
                                                    in_=self.ps[b][:, 0:NT * H].rearrange("p (k h) -> p k h", h=H)),
                reads=[("ps", b)], writes=["cbias"])

    def fox_load(self, pair):
        for i in range(2):
            hd = 2 * pair + i
            self.op("sp", lambda e, i=i, hd=hd: e.dma_start(out=self.KT[i][0:65, :], in_=self.Ks[hd]),
                    reads=[("Ks", hd), ("Ks1", hd)], writes=[("KT", i, tb) for tb in range(NB)], dsem=f"kt{i}")
            self.op("sp", lambda e, i=i, hd=hd: e.dma_start(out=self.QT[i][64:65, :], in_=self.Cs[hd:hd + 1, :]),
                    reads=["Cs"], writes=[("QTaug", i)], dsem=f"qa{i}")
        self.op("sp", lambda e, pair=pair: e.dma_start(out=self.Vp[:, :, :].rearrange("p t f -> p (t f)"), in_=self.Vs[pair]),
                reads=[("Vs", pair)], writes=[("Vp", g, i) for g in range(4) for i in range(2)], dsem="vp")

    def final(self):
        self.stats()
        for tt in range(NT):
            k = tt % 2
            self.op("dve", lambda e, tt=tt, k=k: e.scalar_tensor_tensor(
                out=self.xs[k], in0=self.h[:, tt, :], scalar=self.rstd[:, tt:tt + 1], in1=self.gfin[:],
                op0=ALU.mult, op1=ALU.mult), reads=[("h", tt), "rstd", "gfin"], writes=self.XS_KEYS[k])
            self.op("sp", lambda e, tt=tt, k=k: e.dma_start(out=self.out[tt * 128:(tt + 1) * 128, :], in_=self.xs[k]),
                    reads=self.XS_KEYS[k], writes=[("out", tt)], dsem=f"o{k}")
        self.finish(["o0", "o1"])

    def dump_h(self):
        for tt in range(NT):
            self.op("sp", lambda e, tt=tt: e.dma_start(out=self.out[tt * 128:(tt + 1) * 128, :], in_=self.h[:, tt, :]),
                    reads=[("h", tt)], writes=[("out", tt)], dsem="o0")
        self.finish(["o0"])

    def dump_dbg(self):
        nc = self.nc
        dv = nc.dram_tensor("dbg_v", [128, NT * 192], BF16, kind="ExternalOutput").ap()
        dq = nc.dram_tensor("dbg_q", [128, 2 * S], BF16, kind="ExternalOutput").ap()
        dk = nc.dram_tensor("dbg_k", [128, 2 * S], BF16, kind="ExternalOutput").ap()
        dr = nc.dram_tensor("dbg_r", [128, NSLOT * SLOT], BF16, kind="ExternalOutput").ap()
        self.op("sp", lambda e: e.dma_start(out=dv, in_=self.Vp[:, :, :].rearrange("p t f -> p (t f)")),
                reads=[("Vp", g, i) for g in range(4) for i in range(2)], dsem="o0")
        self.op("sp", lambda e: e.dma_start(out=dq, in_=self.QTall[:, :]),
                reads=self.XS_KEYS[0] + self.XS_KEYS[1], dsem="o0")
        self.op("sp", lambda e: e.dma_start(out=dk, in_=self.KTall[:, :]),
                reads=[("KT", i, tb) for i in range(2) for tb in range(NB)], dsem="o0")
        self.op("sp", lambda e: e.dma_start(out=dr, in_=self.ring[:, :]),
                reads=[("ring", s_, t) for s_ in range(NSLOT) for t in range(3)], dsem="o0")
        self.dump_h()

    def finish(self, keys):
        sp = self.engs["sp"]
        for k in list(keys) + [k for k in self.dcount if k not in keys]:
            if self.dcount[k] > 0:
                sp.h.wait_ge(self.sems[k], self.dcount[k])

    def build(self):
        with self.es:
            self.declare()
            self.setup()
            self.build_wlist()
            self._body()
        return self.nc

    def _body(self):
        for L in range(DEPTH):
            if L == N_A:
                self.shared_kv()
                if self.stop == "kv":
                    return self.dump_h()
            if self.stop == "setup":
                return self.dump_h()
            self.norm(L)
            if self.stop == "norm":
                return self.dump_h()
            self.gset = self.G_ATTN
            for pair in (self.pair_order if L < N_A else range(8)):
                if L < N_A:
                    s, wv = self.wget("qkv")
                    self.wreads = [("ring", s, 0)]
                    self.proj_T(wv[0], 0, self.QT, "QT")
                    self.wreads = [("ring", s, 1)]
                    self.proj_T(wv[1], 0, self.KT, "KT")
                    self.wreads = [("ring", s, 2)]
                    self.proj_V(wv[2], 0)
                    self.wdone()
                    if self.stop == "proj":
                        return self.dump_h()
                    self.moba_gate(pair)
                    grp = [(i, j) for j in (0, 1) for i in range(2)] + [(i, j) for j in (2, 3) for i in range(2)]
                    self.attention(pair, 72, self.abias, "abias", groups=grp,
                                   hook=lambda pair=pair: self.moba_gate2(pair))
                    if self.stop == "att":
                        return self.dump_h()
                else:
                    s, wv = self.wget("q")
                    self.wreads = [("ring", s, 0)]
                    self.fox_load(pair)
                    self.proj_T(wv[0], 0, self.QT, "QT")
                    self.wdone()
                    self.attention(pair, 65, self.cbias, "cbias")
            self.gset = self.G_FULL
            self.wo()
            if self.stop == f"attn{L}":
                return self.dump_h()
            self.norm(4 + L)
            self.mlp()
            if self.stop == f"mlp{L}":
                return self.dump_h()
        self.final()


def _consts():
    bf = ml_dtypes.bfloat16
    slopes = (2.0 ** (-8.0 * np.arange(1, H + 1) / H)).astype(np.float32)
    p = np.arange(128)
    c = {}
    c["identf"] = np.eye(128, dtype=np.float32)
    c["identb"] = np.eye(128, dtype=np.float32).astype(bf)
    c["trimask"] = np.where(p[:, None] > p[None, :], NEG, 0.0).astype(np.float32).astype(bf)
    c["kblock"] = (np.arange(S)[None, :] // 256 == np.arange(8)[:, None]).astype(np.float32).astype(bf)
    pos = (128 * np.arange(NT)[None, :] + p[:, None]).astype(np.float32)
    c["abias"] = np.ascontiguousarray((pos[:, :, None] * slopes[None, None, :]).astype(np.float32))
    aq = (-(1.0 / SCALE) * slopes[:, None] * np.arange(S, dtype=np.float32)[None, :]).astype(np.float32)
    c["augq"] = np.ascontiguousarray(np.broadcast_to(aq[:, None, :], (H, 8, S))).astype(bf)
    gm = np.zeros((128, 8, 2, 8), np.float32)
    gv = np.zeros((128, 8, 2, 8), np.float32)
    for qq in range(8):
        b = (8 + qq) // 2
        gm[:, qq, :, b:] = -1e30
        gv[:, qq, :, :b] = 1.0
    c["gmask"] = gm.reshape(128, 128)
    c["gvalid"] = gv.reshape(128, 128)
    return c


_CACHE = {}


def _get_nc(stop=None):
    if stop not in _CACHE:
        _CACHE[stop] = B(stop).build()
    return _CACHE[stop]


def _in_maps(inputs, cores):
    f = lambda a: np.ascontiguousarray(np.asarray(a, dtype=np.float32))
    gt = np.concatenate([f(inputs["attn_norm"]), f(inputs["mlp_norm"]), f(inputs["kv_norm"])[None, :]], axis=0)
    gtab = np.ascontiguousarray(gt.reshape(9, KC, 128).transpose(2, 0, 1))
    def pairwise(w, nt):
        lead = w.shape[:-2]
        w = w.reshape(lead + (KC, 128, nt, 8, 128))
        nl = len(lead)
        perm = tuple(range(nl)) + (nl + 3, nl + 1, nl + 2, nl + 0, nl + 4)
        return np.ascontiguousarray(w.transpose(perm)).reshape(lead + (8, 128, nt * KC * 128))
    shared = {
        "wqkv_r": pairwise(f(inputs["moba_w_qkv"]), 3), "wq_r": pairwise(f(inputs["fox_w_q"]), 1), "w_o": f(inputs["w_o"]),
        "wkv_r": pairwise(f(inputs["w_kv"]), 2),
        "wf_r": np.ascontiguousarray(f(inputs["w_f"]).reshape(KC, 128, H).transpose(1, 0, 2)).reshape(128, KC * H),
        "b_f": f(inputs["b_f"]).reshape(H, 1),
        "w_up": f(inputs["w_up"]), "w_down": f(inputs["w_down"]), "final_norm": f(inputs["final_norm"]),
        "gtab": gtab,
    }
    shared.update(_consts())
    x = f(inputs["x"])
    return [dict(shared, x=np.ascontiguousarray(x[b])) for b in cores]


def kernel(**inputs):
    nc = _get_nc(None)
    maps = _in_maps(inputs, list(range(8)))
    res = run_bass_kernel_spmd(nc, maps, core_ids=list(range(8)))
    return np.stack([np.asarray(r["out"], dtype=np.float32) for r in res.results], axis=0)
```
